# Optimizing a Trainium2 kernel written in Bass

```python
import math
import jax, jax.numpy as jnp
from jax import lax
import numpy as np

D_MODEL = 1024
BATCH = 8
SEQ = 4096
DEPTH = 1

POOL_WINDOWS = (2, 4, 8, 16)
N_POOL_GROUPS = len(POOL_WINDOWS)
POOL_WIDTH = D_MODEL // 2
POOL_GROUP = POOL_WIDTH // N_POOL_GROUPS

MLA_HEADS = 8
QK_NOPE = 64
QK_ROPE = 32
QK_DIM = QK_NOPE + QK_ROPE
V_HEAD = 64
MLA_WIDTH = MLA_HEADS * V_HEAD
Q_LORA = 256
KV_LORA = 128
ROPE_THETA = 10000.0
Q_BLOCK = 128

IN_WIDTH = POOL_WIDTH + Q_LORA + KV_LORA + QK_ROPE
MIX_WIDTH = POOL_WIDTH + MLA_WIDTH

N_EXPERTS = 64
TOP_K = 8
N_GROUPS = 8
TOPK_GROUPS = 4
D_EXPERT = 256
D_SHARED = 256
ROUTED_SCALE = 2.5
MOE_BLOCK = 128

ALPHA = (2 * DEPTH) ** 0.25
BETA = (8 * DEPTH) ** -0.25
LN_EPS = 1e-5
RMS_EPS = 1e-6

kernel_name = "hybrid_pool_mla_moe_deepnorm"


def layer_norm(x, g, b):
    xf = x.astype(jnp.float32)
    mu = jnp.mean(xf, axis=-1, keepdims=True)
    xc = xf - mu
    var = jnp.mean(xc * xc, axis=-1, keepdims=True)
    return (xc * lax.rsqrt(var + LN_EPS) * g + b).astype(x.dtype)


def rms_norm(x, g):
    xf = x.astype(jnp.float32)
    ms = jnp.mean(xf * xf, axis=-1, keepdims=True)
    return (xf * lax.rsqrt(ms + RMS_EPS) * g).astype(x.dtype)


def window_mean(u, w):
    B, S, C = u.shape
    left = w // 2
    right = w - 1 - left
    c = jnp.concatenate([jnp.zeros((B, 1, C), jnp.float32),
                         jnp.cumsum(u.astype(jnp.float32), axis=1)], axis=1)
    pos = jnp.arange(S)
    lo = jnp.clip(pos - left, 0, S)
    hi = jnp.clip(pos + right + 1, 0, S)
    count = (hi - lo).astype(jnp.float32)
    mean = (c[:, hi] - c[:, lo]) / count[None, :, None]
    return mean.astype(u.dtype)


def pool_mixer(u, w_pool, pool_scale):
    B, S, _ = u.shape
    ug = u.reshape(B, S, N_POOL_GROUPS, POOL_GROUP)
    pooled = jnp.stack([window_mean(ug[:, :, g], w) for g, w in enumerate(POOL_WINDOWS)], axis=2) - ug
    y = jnp.einsum('bsgc,gcd->bsgd', pooled, w_pool)
    return y.reshape(B, S, POOL_WIDTH) * pool_scale


def apply_rope(t, cos, sin):
    t1, t2 = jnp.split(t, 2, axis=-1)
    return jnp.concatenate([t1 * cos - t2 * sin, t2 * cos + t1 * sin], axis=-1)


def mla(q_lat, kv_lat, k_rope_in, q_norm_g, w_q_up, kv_norm_g, w_kv_up):
    B, S, _ = q_lat.shape
    q = (rms_norm(q_lat, q_norm_g) @ w_q_up).reshape(B, S, MLA_HEADS, QK_DIM)
    kv = (rms_norm(kv_lat, kv_norm_g) @ w_kv_up).reshape(B, S, MLA_HEADS, QK_NOPE + V_HEAD)
    k_nope, v = kv[..., :QK_NOPE], kv[..., QK_NOPE:]

    pos = jnp.arange(S, dtype=jnp.float32)
    inv_freq = ROPE_THETA ** (-jnp.arange(0, QK_ROPE, 2, dtype=jnp.float32) / QK_ROPE)
    ang = pos[:, None] * inv_freq[None, :]
    cos = jnp.cos(ang)[:, None, :].astype(q.dtype)
    sin = jnp.sin(ang)[:, None, :].astype(q.dtype)

    q = jnp.concatenate([q[..., :QK_NOPE], apply_rope(q[..., QK_NOPE:], cos, sin)], axis=-1)
    k_rope = apply_rope(k_rope_in[:, :, None, :], cos, sin)
    k = jnp.concatenate([k_nope, jnp.broadcast_to(k_rope, (B, S, MLA_HEADS, QK_ROPE))], axis=-1)

    scale = QK_DIM ** -0.5
    n_blk = S // Q_BLOCK
    q_blocks = q.reshape(B, n_blk, Q_BLOCK, MLA_HEADS, QK_DIM).transpose(1, 0, 2, 3, 4)

    def attend(qb):
        s = jnp.einsum('bqhd,bkhd->bhqk', qb, k, preferred_element_type=jnp.float32) * scale
        p = jax.nn.softmax(s, axis=-1).astype(v.dtype)
        return jnp.einsum('bhqk,bkhd->bqhd', p, v)

    o = lax.map(attend, q_blocks)
    return o.transpose(1, 0, 2, 3, 4).reshape(B, S, MLA_WIDTH)


def moe(h, w_router, router_bias, w_gate, w_up, w_down, w_sh_gate, w_sh_up, w_sh_down):
    B, S, D = h.shape
    xf = h.reshape(-1, D)
    N = xf.shape[0]
    NK = N * TOP_K

    logits = jnp.einsum('nd,de->ne', xf, w_router, preferred_element_type=jnp.float32)
    scores = jax.nn.sigmoid(logits)
    biased = scores + router_bias.astype(jnp.float32)
    grp_score = lax.top_k(biased.reshape(N, N_GROUPS, N_EXPERTS // N_GROUPS), 2)[0].sum(-1)
    _, top_grp = lax.top_k(grp_score, TOPK_GROUPS)
    grp_mask = jnp.any(top_grp[..., None] == jnp.arange(N_GROUPS), axis=1)
    expert_mask = jnp.repeat(grp_mask, N_EXPERTS // N_GROUPS, axis=1)
    _, top_idx = lax.top_k(jnp.where(expert_mask, biased, -jnp.inf), TOP_K)
    top_w = jnp.take_along_axis(scores, top_idx, axis=1)
    top_w = top_w / jnp.sum(top_w, axis=-1, keepdims=True) * ROUTED_SCALE

    flat_e = top_idx.reshape(-1)
    order = jnp.argsort(flat_e)
    sorted_e = flat_e[order]
    tok = order // TOP_K
    sizes = jnp.bincount(flat_e, length=N_EXPERTS)
    padded = (sizes + MOE_BLOCK - 1) // MOE_BLOCK * MOE_BLOCK
    pad_end = jnp.cumsum(padded)
    pad_start = pad_end - padded
    start = jnp.cumsum(sizes) - sizes
    dest = pad_start[sorted_e] + jnp.arange(NK) - start[sorted_e]
    n_blocks = -(-(NK + N_EXPERTS * (MOE_BLOCK - 1)) // MOE_BLOCK)
    P = n_blocks * MOE_BLOCK

    slot_tok = jnp.full((P,), N, jnp.int32).at[dest].set(tok.astype(jnp.int32))
    slot_w = jnp.zeros((P,), jnp.float32).at[dest].set(top_w.reshape(-1)[order])
    x_pad = jnp.concatenate([xf, jnp.zeros((1, D), xf.dtype)], axis=0)
    buf = x_pad[slot_tok].reshape(n_blocks, MOE_BLOCK, D)
    block_e = jnp.minimum(jnp.searchsorted(pad_end, jnp.arange(n_blocks) * MOE_BLOCK, side='right'),
                          N_EXPERTS - 1)

    def expert_block(args):
        xb, e = args
        return (jax.nn.silu(xb @ w_gate[e]) * (xb @ w_up[e])) @ w_down[e]

    out_buf = lax.map(expert_block, (buf, block_e)).reshape(P, D)
    routed = jnp.zeros((N + 1, D), xf.dtype).at[slot_tok].add(
        out_buf * slot_w[:, None].astype(xf.dtype))[:N]
    shared = (jax.nn.silu(xf @ w_sh_gate) * (xf @ w_sh_up)) @ w_sh_down
    return (routed + shared).reshape(B, S, D)


def setup_inputs(seed: int = 0) -> dict:
    key = jax.random.key(seed)
    ks = jax.random.split(key, 24)
    L = DEPTH

    def nrm(k, shape, scale):
        return jax.random.normal(k, shape, jnp.float32) * scale

    v_scale = jnp.concatenate([jnp.ones((QK_NOPE,), jnp.float32), jnp.full((V_HEAD,), BETA, jnp.float32)])
    w_kv_up = (nrm(ks[6], (L, KV_LORA, MLA_HEADS, QK_NOPE + V_HEAD), KV_LORA ** -0.5) * v_scale
               ).reshape(L, KV_LORA, MLA_HEADS * (QK_NOPE + V_HEAD))
    return {
        "x": nrm(ks[0], (BATCH, SEQ, D_MODEL), 1.0),
        "w_in": nrm(ks[1], (L, D_MODEL, IN_WIDTH), D_MODEL ** -0.5),
        "w_pool": nrm(ks[2], (L, N_POOL_GROUPS, POOL_GROUP, POOL_GROUP), POOL_GROUP ** -0.5),
        "pool_scale": 1.0 + nrm(ks[3], (L, POOL_WIDTH), 0.1),
        "q_norm_g": 1.0 + nrm(ks[4], (L, Q_LORA), 0.02),
        "w_q_up": nrm(ks[5], (L, Q_LORA, MLA_HEADS * QK_DIM), Q_LORA ** -0.5),
        "kv_norm_g": 1.0 + nrm(ks[7], (L, KV_LORA), 0.02),
        "w_kv_up": w_kv_up,
        "w_out": nrm(ks[8], (L, MIX_WIDTH, D_MODEL), MIX_WIDTH ** -0.5 * BETA),
        "ln1_g": 1.0 + nrm(ks[9], (L, D_MODEL), 0.02),
        "ln1_b": nrm(ks[10], (L, D_MODEL), 0.02),
        "w_router": nrm(ks[11], (L, D_MODEL, N_EXPERTS), D_MODEL ** -0.5),
        "router_bias": nrm(ks[12], (L, N_EXPERTS), 0.01),
        "w_gate": nrm(ks[13], (L, N_EXPERTS, D_MODEL, D_EXPERT), D_MODEL ** -0.5),
        "w_up": nrm(ks[14], (L, N_EXPERTS, D_MODEL, D_EXPERT), D_MODEL ** -0.5),
        "w_down": nrm(ks[15], (L, N_EXPERTS, D_EXPERT, D_MODEL), D_EXPERT ** -0.5 * BETA),
        "w_sh_gate": nrm(ks[16], (L, D_MODEL, D_SHARED), D_MODEL ** -0.5),
        "w_sh_up": nrm(ks[17], (L, D_MODEL, D_SHARED), D_MODEL ** -0.5),
        "w_sh_down": nrm(ks[18], (L, D_SHARED, D_MODEL), D_SHARED ** -0.5 * BETA),
        "ln2_g": 1.0 + nrm(ks[19], (L, D_MODEL), 0.02),
        "ln2_b": nrm(ks[20], (L, D_MODEL), 0.02),
    }


def reference(x, w_in, w_pool, pool_scale, q_norm_g, w_q_up, kv_norm_g, w_kv_up, w_out,
              ln1_g, ln1_b, w_router, router_bias, w_gate, w_up, w_down,
              w_sh_gate, w_sh_up, w_sh_down, ln2_g, ln2_b):
    h = x
    splits = [POOL_WIDTH, POOL_WIDTH + Q_LORA, POOL_WIDTH + Q_LORA + KV_LORA]
    for l in range(DEPTH):
        proj = h @ w_in[l]
        u_pool, q_lat, kv_lat, k_rope = jnp.split(proj, splits, axis=-1)
        y_pool = pool_mixer(u_pool, w_pool[l], pool_scale[l])
        y_mla = mla(q_lat, kv_lat, k_rope, q_norm_g[l], w_q_up[l], kv_norm_g[l], w_kv_up[l])
        mixed = jnp.concatenate([y_pool, y_mla], axis=-1) @ w_out[l]
        h = layer_norm(ALPHA * h + mixed, ln1_g[l], ln1_b[l])
        f = moe(h, w_router[l], router_bias[l], w_gate[l], w_up[l], w_down[l],
                w_sh_gate[l], w_sh_up[l], w_sh_down[l])
        h = layer_norm(ALPHA * h + f, ln2_g[l], ln2_b[l])
    return h
```

```python
import numpy as np
import ml_dtypes
import concourse.bass as bass
import concourse.mybir as mybir
from concourse.bass_utils import run_bass_kernel_spmd

F32 = mybir.dt.float32
BF16 = mybir.dt.bfloat16
I32 = mybir.dt.int32
AF = mybir.ActivationFunctionType
ALU = mybir.AluOpType
AX = mybir.AxisListType

ENGS = ("pe", "act", "dve", "pool", "sp")
NDMASEM = 8
SEM_ROLL = 12000

S_LEN = 4096
DM = 1024
NT = S_LEN // 128
C_CAP = 768
NSLOT = 64 * C_CAP
ALPHA = 2.0 ** 0.25
QK_SCALE = 96.0 ** -0.5
LN_EPS = 1e-5
RMS_EPS = 1e-6
POOL_WINDOWS = (2, 4, 8, 16)


class _Buf:
    __slots__ = ("ws", "rs")

    def __init__(self):
        self.ws = []
        self.rs = []


class Sched:
    def __init__(self, nc):
        self.nc = nc
        self.ops = []
        self.streams = {e: [] for e in ENGS}
        self.bufs = {}

    def _buf(self, x):
        name = x if isinstance(x, str) else x.tensor.name
        b = self.bufs.get(name)
        if b is None:
            b = self.bufs[name] = _Buf()
        return b

    def alias(self, new_name, old_names):
        b = self._buf(new_name)
        for o in old_names:
            ob = self.bufs.get(o)
            if ob is None:
                continue
            b.ws = list(dict.fromkeys(b.ws + ob.ws))
            b.rs = list(dict.fromkeys(b.rs + ob.rs))

    def op(self, eng, fn, reads=(), writes=(), scat=(), dma=False):
        i = len(self.ops)
        deps = set()

        def uniq(xs):
            out = []
            for x in xs:
                b = self._buf(x)
                if not any(b is o for o in out):
                    out.append(b)
            return out

        rb, wb, sb = uniq(reads), uniq(writes), uniq(scat)
        for b in rb:
            deps.update(b.ws)
        for b in wb:
            deps.update(b.ws)
            deps.update(b.rs)
        for b in sb:
            deps.update(b.rs)
        for b in rb:
            if not dma:
                b.rs = [j for j in b.rs if self.ops[j][3] or self.ops[j][0] != eng]
            b.rs.append(i)
        for b in wb:
            b.ws = [i]
            b.rs = []
        for b in sb:
            if not dma:
                b.ws = [j for j in b.ws if self.ops[j][3] or self.ops[j][0] != eng]
            b.ws.append(i)
        deps.discard(i)
        self.ops.append([eng, fn, deps, dma, False, None])
        self.streams[eng].append(i)
        return i

    def dma(self, q, out, in_, reads=None, writes=None, scat=(), **kw):
        r = [in_] if reads is None else reads
        w = [out] if writes is None else writes
        return self.op(q, lambda e: e.dma_start(out=out, in_=in_, **kw), r, w, scat, dma=True)

    def dmaT(self, q, out, in_):
        return self.op(q, lambda e: e.dma_start_transpose(out=out, in_=in_), [in_], [out], dma=True)

    def mm(self, out, lhsT, rhs, start=True, stop=True):
        return self.op("pe", lambda e: e.matmul(out, lhsT, rhs, start=start, stop=stop), [lhsT, rhs], [out])

    def transpose(self, out, in_, ident):
        return self.op("pe", lambda e: e.transpose(out, in_, ident), [in_, ident], [out])

    def act(self, out, in_, func, bias=None, scale=None, accum_out=None, scat=False):
        kw = {}
        r = [in_]
        w = [out]
        if bias is not None:
            kw["bias"] = bias
            if not isinstance(bias, (int, float)):
                r.append(bias)
        if scale is not None:
            kw["scale"] = scale
            if not isinstance(scale, (int, float)):
                r.append(scale)
        if accum_out is not None:
            kw["accum_out"] = accum_out
            w.append(accum_out)
        if scat:
            return self.op("act", lambda e: e.activation(out, in_, func, **kw), r, [], w)
        return self.op("act", lambda e: e.activation(out, in_, func, **kw), r, w)

    def tt(self, eng, out, in0, in1, op, scat=False):
        if scat:
            return self.op(eng, lambda e: e.tensor_tensor(out, in0, in1, op), [in0, in1], [], [out])
        return self.op(eng, lambda e: e.tensor_tensor(out, in0, in1, op), [in0, in1], [out])

    def ts(self, eng, out, in0, s1, s2, op0, op1=None, accum_out=None, scat=False):
        r = [in0]
        w = [out]
        if not isinstance(s1, (int, float)):
            r.append(s1)
        if s2 is not None and not isinstance(s2, (int, float)):
            r.append(s2)
        kw = {}
        if op1 is not None:
            kw["op1"] = op1
        if accum_out is not None:
            kw["accum_out"] = accum_out
            w.append(accum_out)
        if scat:
            return self.op(eng, lambda e: e.tensor_scalar(out, in0, s1, s2, op0, **kw), r, [], w)
        return self.op(eng, lambda e: e.tensor_scalar(out, in0, s1, s2, op0, **kw), r, w)

    def stt(self, eng, out, in0, scalar, in1, op0, op1, accum_out=None, scat_acc=False):
        r = [in0, in1]
        if not isinstance(scalar, (int, float)):
            r.append(scalar)
        kw = {}
        w = [out]
        sc = []
        if accum_out is not None:
            kw["accum_out"] = accum_out
            if scat_acc:
                sc.append(accum_out)
            else:
                w.append(accum_out)
        return self.op(eng, lambda e: e.scalar_tensor_tensor(out, in0, scalar, in1, op0, op1, **kw), r, w, sc)

    def copy(self, eng, out, in_, scat=False):
        if eng == "act":
            fn = lambda e: e.copy(out, in_)
        else:
            fn = lambda e: e.tensor_copy(out, in_)
        if scat:
            return self.op(eng, fn, [in_], [], [out])
        return self.op(eng, fn, [in_], [out])

    def memset(self, eng, ap, val):
        return self.op(eng, lambda e: e.memset(ap, val), [], [ap])

    def reduce(self, eng, out, in_, op, axis=AX.X):
        return self.op(eng, lambda e: e.tensor_reduce(out, in_, axis, op), [in_], [out])

    def emit(self):
        nc = self.nc
        ops = self.ops
        for o in ops:
            for d in o[2]:
                p = ops[d]
                if p[3]:
                    continue
                if p[0] == "pe" and o[0] == "pe" and not o[3]:
                    continue
                p[4] = True
        dsem = {e: [nc.alloc_semaphore(f"dsem_{e}_{k}") for k in range(NDMASEM)] for e in ("sp", "act", "pool")}
        dcnt = {e: [0] * NDMASEM for e in dsem}
        drr = {e: 0 for e in dsem}
        prevdma = {}
        nsig = {}
        for e in ENGS:
            cur = None
            cnt = 0
            n = 0
            for i in self.streams[e]:
                o = ops[i]
                if o[3]:
                    k = drr[e]
                    drr[e] = (k + 1) % NDMASEM
                    prevdma[i] = (dsem[e][k], dcnt[e][k])
                    dcnt[e][k] += 16
                    o[5] = (dsem[e][k], dcnt[e][k])
                elif o[4]:
                    if cur is None or cnt >= SEM_ROLL:
                        cur = nc.alloc_semaphore(f"sem_{e}_{n}")
                        n += 1
                        cnt = 0
                    cnt += 1
                    o[5] = (cur, cnt)
            nsig[e] = (n, cnt)
        self.stats = dict(nops=len(ops), per_eng={e: len(self.streams[e]) for e in ENGS}, nsem=nsig)
        streams = self.streams

        def run(e, engobj):
            known = {}

            def wait(sem, val):
                if val <= 0 or known.get(sem.num, 0) >= val:
                    return
                engobj.wait_ge(sem, val)
                known[sem.num] = val

            for i in streams[e]:
                o = ops[i]
                for d in sorted(o[2]):
                    p = ops[d]
                    if p[5] is None:
                        continue
                    if (not p[3]) and p[0] == "pe" and e == "pe" and not o[3]:
                        continue
                    wait(*p[5])
                if o[3]:
                    wait(*prevdma[i])
                ins = o[1](engobj)
                if o[3]:
                    ins.then_inc(o[5][0], 16)
                elif o[4]:
                    ins.then_inc(o[5][0], 1)
            if e in dsem:
                for k in range(NDMASEM):
                    wait(dsem[e][k], dcnt[e][k])

        with nc.Block() as block:
            @block.sync
            def _(eng):
                run("sp", eng)

            @block.scalar
            def _(eng):
                run("act", eng)

            @block.vector
            def _(eng):
                run("dve", eng)

            @block.gpsimd
            def _(eng):
                run("pool", eng)

            @block.tensor
            def _(eng):
                run("pe", eng)


class Alloc:
    def __init__(self, nc, S):
        self.nc = nc
        self.S = S
        self.lo = (nc.sbuf_base + 63) // 64 * 64
        self.hi = nc.sbuf_top
        self.ptr = self.lo
        self.live = []
        self.peak = 0

    def __call__(self, name, shape, dtype):
        per = 1
        for s in shape[1:]:
            per *= s
        nbytes = per * {F32: 4, BF16: 2, I32: 4}[dtype]
        nbytes = (nbytes + 63) // 64 * 64
        start = self.ptr
        end = start + nbytes
        assert end <= self.hi, f"SBUF overflow allocating {name}: {end} > {self.hi}"
        t = self.nc.alloc_sbuf_tensor_at(name, list(shape), dtype, offset=start)
        old = [n for (s, e, n) in self.live if s < end and e > start]
        if old:
            self.S.alias(t.name, old)
        self.live.append((start, end, t.name))
        self.ptr = end
        self.peak = max(self.peak, end)
        return t

    def mark(self):
        return self.ptr

    def reset(self, m):
        self.ptr = m


def _host_constants():
    pos = np.arange(S_LEN, dtype=np.float32)
    inv_freq = (10000.0 ** (-np.arange(0, 32, 2, dtype=np.float32) / 32)).astype(np.float32)
    ang = pos[:, None] * inv_freq[None, :]
    cos = np.cos(ang).astype(np.float32).T
    sin = np.sin(ang).astype(np.float32).T
    cs = np.stack([np.concatenate([cos, cos], 0), np.concatenate([-sin, sin], 0)], 0).astype(np.float32)

    band = np.zeros((4, 5, 128, 128), np.float32)

    def blk(w, t_src, t_dst):
        left = w // 2
        right = w - 1 - left
        s = np.arange(t_dst * 128, t_dst * 128 + 128)
        sp = np.arange(t_src * 128, t_src * 128 + 128)
        lo = np.clip(s - left, 0, S_LEN)
        hi = np.clip(s + right + 1, 0, S_LEN)
        cntv = (hi - lo).astype(np.float32)
        m = ((sp[:, None] >= lo[None, :]) & (sp[:, None] < hi[None, :])).astype(np.float32) / cntv[None, :]
        m = m - (sp[:, None] == s[None, :]).astype(np.float32)
        return m

    for g, w in enumerate(POOL_WINDOWS):
        band[g, 0] = blk(w, 4, 5)
        band[g, 1] = blk(w, 5, 5)
        band[g, 2] = blk(w, 6, 5)
        band[g, 3] = blk(w, 0, 0)
        band[g, 4] = blk(w, NT - 1, NT - 1)
    tri = np.triu(np.ones((128, 128), np.float32), 1)
    eoff1 = (np.arange(64, dtype=np.float32) * C_CAP + 1.0).astype(np.float32)
    return {
        "cs": cs,
        "band": band.astype(ml_dtypes.bfloat16),
        "identf": np.eye(128, dtype=np.float32),
        "tri": tri.astype(ml_dtypes.bfloat16),
        "eoff1": eoff1,
    }


def build(debug=False, stop_after=99):
    nc = bass.Bass("TRN2", target_bir_lowering=False)

    def DI(name, shape, dt):
        return nc.dram_tensor(name, list(shape), dt, kind="ExternalInput").ap()

    def DS(name, shape, dt):
        return nc.dram_tensor(name, list(shape), dt, kind=("ExternalOutput" if debug else "Internal")).ap()

    x = DI("x", [S_LEN, DM], F32)
    w_in = DI("w_in", [DM, 928], F32)
    w_pool = DI("w_pool", [4, 128, 128], F32)
    pool_scale = DI("pool_scale", [512], F32)
    q_norm_g = DI("q_norm_g", [256], F32)
    w_q_up = DI("w_q_up", [256, 768], F32)
    kv_norm_g = DI("kv_norm_g", [128], F32)
    w_kv_up = DI("w_kv_up", [128, 1024], F32)
    w_out = DI("w_out", [1024, 1024], F32)
    ln1_g = DI("ln1_g", [1024], F32)
    ln1_b = DI("ln1_b", [1024], F32)
    w_router = DI("w_router", [1024, 64], F32)
    router_bias = DI("router_bias", [64], F32)
    w_gate = DI("w_gate", [64, 1024, 256], F32)
    w_up = DI("w_up", [64, 1024, 256], F32)
    w_down = DI("w_down", [64, 256, 1024], F32)
    w_sh_gate = DI("w_sh_gate", [1024, 256], F32)
    w_sh_up = DI("w_sh_up", [1024, 256], F32)
    w_sh_down = DI("w_sh_down", [256, 1024], F32)
    ln2_g = DI("ln2_g", [1024], F32)
    ln2_b = DI("ln2_b", [1024], F32)
    cs_d = DI("cs", [2, 32, S_LEN], F32)
    band_d = DI("band", [4, 5, 128, 128], BF16)
    identf_d = DI("identf", [128, 128], F32)
    tri_d = DI("tri", [128, 128], BF16)
    eoff1_d = DI("eoff1", [64], F32)
    out = nc.dram_tensor("out", [S_LEN, DM], F32, kind="ExternalOutput").ap()

    yp_d = DS("yp_d", [4, 128, S_LEN], BF16)
    h1_d = DS("h1_d", [S_LEN, DM], F32)
    acc_d = DS("acc_d", [S_LEN, DM], F32)
    xbuf_d = nc.dram_tensor("xbuf_d", [NSLOT + 1, DM], BF16, kind="Internal").ap()
    ybuf_d = nc.dram_tensor("ybuf_d", [NSLOT + 1, DM], BF16, kind="Internal").ap()
    if debug:
        kt_dbg = DS("kt_dbg", [96, 8 * S_LEN], BF16)
        v_dbg = DS("v_dbg", [128, NT * 8 * 65], BF16)
        qn_dbg = DS("qn_dbg", [128, 2 * S_LEN], BF16)
        idx_dbg = DS("idx_dbg", [128, NT * 8], I32)
        wk_dbg = DS("wk_dbg", [128, NT * 8], F32)
        ym_dbg = DS("ym_dbg", [64, 8, S_LEN], BF16)
        mix_dbg = DS("mix_dbg", [S_LEN, DM], F32)

    S = Sched(nc)
    A = Alloc(nc, S)
    _bc = {}

    def bc_reg(e):
        if "r" not in _bc:
            r_ = e.alloc_register("bcreg")
            e.reg_mov(r_, NSLOT)
            _bc["r"] = r_
        return _bc["r"]
    ps = [nc.alloc_psum_tensor(f"ps{i}", [128, 512], F32) for i in range(8)]
    rr = [0]

    def nb():
        p = ps[rr[0] % 8]
        rr[0] += 1
        return p

    identf = A("identf_sb", [128, 128], F32)
    ones_f = A("ones_f", [128, 128], F32)
    ones_b = A("ones_b", [128, 128], BF16)
    idx_all = A("idx_all", [128, NT * 8], I32)
    wk_all = A("wk_all", [128, NT * 8], F32)
    S.dma("sp", identf[:], identf_d)
    S.memset("pool", ones_f[:], 1.0)
    S.memset("pool", ones_b[:], 1.0)
    S.memset("pool", wk_all[:], 0.0)
    base_mark = A.mark()

    KT = A("KT", [128, 8, S_LEN], BF16)
    V_tm = A("V_tm", [128, NT, 8, 65], BF16)
    qnT = A("qnT", [128, 2, S_LEN], BF16)
    wq_sb = A("wq_sb", [128, 2, 768], BF16)
    wq_sw = A("wq_sw", [128, 2, 768], BF16)
    S.memset("pool", V_tm[:], 1.0)
    att_mark = A.mark()

    w_in_sb = A("w_in_sb", [128, 8, 928], BF16)
    wsw = A("wsw", [128, 8, 96], BF16)
    wpool_sb = A("wpool_sb", [128, 4, 128], BF16)
    pscale = A("pscale", [128, 4], F32)
    band_sb = A("band_sb", [128, 20, 128], BF16)
    wkv_sb = A("wkv_sb", [128, 1024], BF16)
    stg = A("stg", [128, 2, 768], F32)
    gq = A("gq", [128, 2], F32)
    gkv = A("gkv", [128, 1], F32)
    xs = [A(f"xs{i}", [128, 1024], F32) for i in range(2)]
    xT = [A(f"xT{i}", [128, 8, 512], BF16) for i in range(1)]
    cs_sb = [A(f"cs_sb{i}", [128, 2, 512], F32) for i in range(1)]
    sq = [A(f"sq{i}", [128, 512], F32) for i in range(2)]
    rq = A("rq", [128, 512], F32)
    rkv = A("rkv", [128, 512], F32)
    kvnT = A("kvnT", [128, 512], BF16)
    u_tm = [A(f"u_tm{i}", [128, 512], BF16) for i in range(10)]
    pooledT = A("pooledT", [128, 4, 512], BF16)
    ypT_c = [A(f"ypT_c{i}", [128, 4, 512], BF16) for i in range(1)]
    t1 = A("t1", [128, 512], F32)
    t2 = A("t2", [128, 512], F32)

    S.dma("pool", w_in_sb[:], w_in.rearrange("(k p) n -> p k n", p=128))
    S.dma("pool", wpool_sb[:], w_pool.rearrange("g c d -> c g d"))
    S.dma("sp", band_sb[:], band_d.rearrange("g k a b -> a (g k) b"))
    for g in range(4):
        S.dma("sp", pscale[:, g:g + 1], pool_scale[g * 128:(g + 1) * 128].rearrange("(p o) -> p o", o=1),
              scat=[pscale[:]], writes=[])
    S.dma("sp", gkv[:], kv_norm_g.rearrange("(p o) -> p o", o=1))
    for k in range(2):
        S.dma("sp", gq[:, k:k + 1], q_norm_g[k * 128:(k + 1) * 128].rearrange("(p o) -> p o", o=1),
              scat=[gq[:]], writes=[])
    S.dma("sp", stg[:, 0, :], w_kv_up[:, 0:768], writes=[], scat=[stg[:]])
    S.dma("sp", stg[:, 1, 0:256], w_kv_up[:, 768:1024], writes=[], scat=[stg[:]])
    S.ts("dve", wkv_sb[:, 0:768], stg[:, 0, :], gkv[:, 0:1], None, ALU.mult, scat=True)
    S.ts("dve", wkv_sb[:, 768:1024], stg[:, 1, 0:256], gkv[:, 0:1], None, ALU.mult, scat=True)
    stg2 = stg
    S.dma("sp", stg2[:], w_q_up.rearrange("(k p) n -> p k n", p=128))
    for k in range(2):
        S.ts("dve", wq_sb[:, k, :], stg2[:, k, :], gq[:, k:k + 1], None, ALU.mult, scat=True)
    wq4 = wq_sb[:].rearrange("p k (h c) -> p k h c", c=96)
    wqs4 = wq_sw[:].rearrange("p k (h c) -> p k h c", c=96)
    for k in range(2):
        S.copy("pool", wqs4[:, k, :, 0:64], wq4[:, k, :, 0:64], scat=True)
        S.copy("pool", wqs4[:, k, :, 64:80], wq4[:, k, :, 80:96], scat=True)
        S.copy("pool", wqs4[:, k, :, 80:96], wq4[:, k, :, 64:80], scat=True)
    S.copy("pool", wsw[:, :, 0:64], w_in_sb[:, :, 832:896], scat=True)
    S.copy("pool", wsw[:, :, 64:80], w_in_sb[:, :, 912:928], scat=True)
    S.copy("pool", wsw[:, :, 80:96], w_in_sb[:, :, 896:912], scat=True)

    def load_x(t):
        S.dma("sp", xs[t % 2][:], x[t * 128:(t + 1) * 128, :])

    def load_cs(buf, c):
        for i in range(2):
            S.dma("sp", buf[64:96, i, :], cs_d[i, :, c * 512:(c + 1) * 512], writes=[], scat=[buf[:]])

    def band(g, kind):
        return band_sb[:, g * 5 + kind, :]

    def pool_chunk(cc):
        yc = ypT_c[0]
        for g in range(4):
            pp = nb()
            for j in range(4):
                t = 4 * cc + j
                srcs = []
                if t > 0:
                    srcs.append((t - 1, band(g, 0)))
                srcs.append((t, band(g, 3 if t == 0 else (4 if t == NT - 1 else 1))))
                if t < NT - 1:
                    srcs.append((t + 1, band(g, 2)))
                for i, (ts_, bm) in enumerate(srcs):
                    S.mm(pp[:, j * 128:(j + 1) * 128], u_tm[ts_ % 10][:, g * 128:(g + 1) * 128], bm,
                         start=(i == 0), stop=(i == len(srcs) - 1))
            S.copy("dve", pooledT[:, g, :], pp[:], scat=True)
            py = nb()
            S.mm(py[:], wpool_sb[:, g, :], pooledT[:, g, :])
            S.act(yc[:, g, :], py[:], AF.Copy, scale=pscale[:, g:g + 1], scat=True)
        S.dma("sp", yp_d.rearrange("g d s -> d g s")[:, :, cc * 512:(cc + 1) * 512], yc[:], writes=[], scat=[yp_d])

    load_x(0)
    load_x(1)
    for c in range(8):
        cols = slice(c * 512, (c + 1) * 512)
        xTc = xT[0]
        csb = cs_sb[0]
        load_cs(csb, c)
        for j in range(4):
            t = 4 * c + j
            xt = xs[t % 2]
            pa, pb = nb(), nb()
            for k in range(8):
                dst = (pa if k < 4 else pb)[:, (k % 4) * 128:(k % 4 + 1) * 128]
                S.transpose(dst, xt[:, k * 128:(k + 1) * 128], identf[:])
            S.act(xTc[:, 0:4, j * 128:(j + 1) * 128], pa[:].rearrange("p (k n) -> p k n", n=128), AF.Copy, scat=True)
            S.copy("dve", xTc[:, 4:8, j * 128:(j + 1) * 128], pb[:].rearrange("p (k n) -> p k n", n=128), scat=True)
            if t + 2 < NT:
                load_x(t + 2)
        pq0, pq1, pkv, prm, prs = nb(), nb(), nb(), nb(), nb()
        for pt, c0 in ((pq0, 512), (pq1, 640), (pkv, 768)):
            for k in range(8):
                S.mm(pt[:], w_in_sb[:, k, c0:c0 + 128], xTc[:, k, :], start=(k == 0), stop=(k == 7))
        for k in range(8):
            S.mm(prm[0:96, :], w_in_sb[:, k, 832:928], xTc[:, k, :], start=(k == 0), stop=(k == 7))
        for k in range(8):
            S.mm(prs[0:96, :], wsw[:, k, :], xTc[:, k, :], start=(k == 0), stop=(k == 7))
        S.act(sq[0][:], pq0[:], AF.Square)
        S.act(sq[1][:], pq1[:], AF.Square)
        pss = nb()
        S.mm(pss[:], ones_f[:], sq[0][:], start=True, stop=False)
        S.mm(pss[:], ones_f[:], sq[1][:], start=False, stop=True)
        S.act(rq[:], pss[:], AF.Ln, scale=1.0 / 256, bias=RMS_EPS)
        S.act(rq[:], rq[:], AF.Exp, scale=-0.5)
        S.tt("dve", qnT[:, 0, cols], pq0[:], rq[:], ALU.mult, scat=True)
        S.tt("dve", qnT[:, 1, cols], pq1[:], rq[:], ALU.mult, scat=True)
        S.act(sq[0][:], pkv[:], AF.Square)
        pss2 = nb()
        S.mm(pss2[:], ones_f[:], sq[0][:])
        S.act(rkv[:], pss2[:], AF.Ln, scale=1.0 / 128, bias=RMS_EPS)
        S.act(rkv[:], rkv[:], AF.Exp, scale=-0.5)
        S.tt("dve", kvnT[:], pkv[:], rkv[:], ALU.mult)
        S.tt("dve", t1[64:96, :], prm[64:96, :], csb[64:96, 0, :], ALU.mult)
        S.tt("dve", t2[64:96, :], prs[64:96, :], csb[64:96, 1, :], ALU.mult)
        S.tt("dve", t1[64:96, :], t1[64:96, :], t2[64:96, :], ALU.add)
        S.copy("pool", KT[64:96, :, cols], t1[64:96, None, :].to_broadcast([32, 8, 512]), scat=True)
        for h in range(8):
            pk = nb()
            S.mm(pk[0:64, :], wkv_sb[:, h * 128:h * 128 + 64], kvnT[:])
            if h % 2 == 0:
                S.act(KT[0:64, h, cols], pk[0:64, :], AF.Copy, scat=True)
            else:
                S.copy("dve", KT[0:64, h, cols], pk[0:64, :], scat=True)
        for j in range(4):
            t = 4 * c + j
            pv = nb()
            S.mm(pv[:], kvnT[:, j * 128:(j + 1) * 128],
                 wkv_sb[:].rearrange("p (h c) -> p h c", c=128)[:, :, 64:128])
            if j % 2 == 0:
                S.act(V_tm[:, t, :, 0:64], pv[:].rearrange("p (h c) -> p h c", c=64), AF.Copy, scat=True)
            else:
                S.copy("dve", V_tm[:, t, :, 0:64], pv[:].rearrange("p (h c) -> p h c", c=64), scat=True)
        for j in range(4):
            t = 4 * c + j
            pu = nb()
            for k in range(8):
                S.mm(pu[:], xTc[:, k, j * 128:(j + 1) * 128], w_in_sb[:, k, 0:512], start=(k == 0), stop=(k == 7))
            S.act(u_tm[t % 10][:], pu[:], AF.Copy)
        if c >= 1:
            pool_chunk(c - 1)
    pool_chunk(7)

    if debug:
        S.dma("sp", kt_dbg, KT[0:96, :, :].rearrange("p h s -> p (h s)"))
        S.dma("sp", v_dbg, V_tm[:].rearrange("p t h c -> p (t h c)"))
        S.dma("sp", qn_dbg, qnT[:].rearrange("p k s -> p (k s)"))

    if stop_after >= 2:
        A.reset(att_mark)
        wout_p = A("wout_p", [128, 4, 1024], BF16)
        wout_m = A("wout_m", [128, 8, 1024], BF16)
        g1_bc = A("g1_bc", [128, 1024], F32)
        b1_bc = A("b1_bc", [128, 1024], F32)
        cs2 = [A(f"cs2_{i}", [128, 2, 512], F32) for i in range(2)]
        qT_h = [A(f"qT_h{i}", [128, 512], BF16) for i in range(2)]
        pT = [A(f"pT{i}", [128, 512], BF16) for i in range(3)]
        tq1 = A("tq1", [128, 512], F32)
        tq2 = A("tq2", [128, 512], F32)
        rec = A("rec", [128, 512], F32)
        bcs = A("bcs", [128, 512], F32)
        ymT = [A(f"ymT{h}", [128, 512], BF16) for h in range(8)]
        ypT_l = A("ypT_l", [128, 4, 512], BF16)
        xs2 = [A(f"xs2_{i}", [128, 1024], F32) for i in range(2)]
        hpre = [A(f"hpre{i}", [128, 1024], F32) for i in range(2)]
        st1 = A("st1", [128, 12], F32)
        mv1 = A("mv1", [128, 2], F32)
        rs1 = A("rs1", [128, 1], F32)

        S.dma("pool", wout_p[:], w_out[0:512, :].rearrange("(g p) n -> p g n", p=128))
        S.dma("pool", wout_m[0:64, :, :], w_out[512:1024, :].rearrange("(h p) n -> p h n", p=64))
        S.dma("sp", g1_bc[:], ln1_g.partition_broadcast(128))
        S.dma("sp", b1_bc[:], ln1_b.partition_broadcast(128))

        S0 = [ps[0], ps[1], ps[2]]
        O_ = [ps[3], ps[4]]
        PA, PB, PC = ps[5], ps[6], ps[7]

        LA = 2
        seq = [(qb, h) for qb in range(8) for h in range(8)]
        deferred = []

        def defer(n, fn):
            deferred.append([n, fn])

        def tick():
            due = [d for d in deferred if d[0] <= 0]
            for d in due:
                deferred.remove(d)
            for d in deferred:
                d[0] -= 1
            for d in due:
                d[1]()

        def flush():
            while deferred:
                tick()

        def emit_qproj(qb, h):
            qcols = slice(qb * 512, (qb + 1) * 512)
            csq = cs2[qb % 2]
            qt = qT_h[h % 2]
            for k in range(2):
                S.mm(PA[0:96, :], wq_sb[:, k, h * 96:(h + 1) * 96], qnT[:, k, qcols], start=(k == 0), stop=(k == 1))
            for k in range(2):
                S.mm(PB[0:96, :], wq_sw[:, k, h * 96:(h + 1) * 96], qnT[:, k, qcols], start=(k == 0), stop=(k == 1))
            S.copy("dve", qt[0:64, :], PA[0:64, :], scat=True)
            S.tt("dve", tq1[64:96, :], PA[64:96, :], csq[64:96, 0, :], ALU.mult)
            S.tt("dve", tq2[64:96, :], PB[64:96, :], csq[64:96, 1, :], ALU.mult)
            S.tt("pool", qt[64:96, :], tq1[64:96, :], tq2[64:96, :], ALU.add, scat=True)

        def emit_norm(qb, h):
            qcols = slice(qb * 512, (qb + 1) * 512)
            po = O_[h % 2]
            S.op("dve", lambda e, po=po: e.reciprocal(rec[64:65, :], po[64:65, :]), [po[:]], [rec[:]])
            S.mm(PC[0:64, :], ones_f[64:65, 0:64], rec[64:65, :])
            S.copy("dve", bcs[0:64, :], PC[0:64, :])
            S.tt("dve", ymT[h][0:64, :], po[0:64, :], bcs[0:64, :], ALU.mult)
            if debug:
                S.dma("sp", ym_dbg[:, h, qcols], ymT[h][0:64, :], writes=[], scat=[ym_dbg])

        def load_mix_inputs(qb):
            qcols = slice(qb * 512, (qb + 1) * 512)
            S.dma("sp", ypT_l[:], yp_d.rearrange("g d s -> d g s")[:, :, qcols])
            S.dma("sp", xs2[0][:], x[(4 * qb) * 128:(4 * qb + 1) * 128, :])

        def emit_mix_tile(qb, j, ln_delay):
            t = 4 * qb + j
            xt = xs2[j % 2]
            if j + 1 < 4:
                S.dma("sp", xs2[(j + 1) % 2][:], x[(t + 1) * 128:(t + 2) * 128, :])
            hp = hpre[t % 2]
            for half, pm in ((0, PA), (1, PB)):
                hc = slice(half * 512, (half + 1) * 512)
                for g in range(4):
                    S.mm(pm[:], ypT_l[:, g, j * 128:(j + 1) * 128], wout_p[:, g, hc], start=(g == 0), stop=False)
                for h in range(8):
                    S.mm(pm[:], ymT[h][0:64, j * 128:(j + 1) * 128], wout_m[0:64, h, hc], start=False, stop=(h == 7))
                if debug:
                    S.copy("dve", rec[:], pm[:])
                    S.dma("sp", mix_dbg[t * 128:(t + 1) * 128, hc], rec[:], writes=[], scat=[mix_dbg])
                S.stt("dve", hp[:, hc], xt[:, hc], ALPHA, pm[:], ALU.mult, ALU.add)
            S.op("dve", lambda e, hp=hp: e.bn_stats(st1[:, 0:6], hp[:, 0:512]), [hp[:]], [st1[:]])
            S.op("dve", lambda e, hp=hp: e.bn_stats(st1[:, 6:12], hp[:, 512:1024]), [hp[:], st1[:]], [st1[:]])
            S.op("dve", lambda e: e.bn_aggr(mv1[:], st1[:]), [st1[:]], [mv1[:]])

            def ln_part(hp=hp, t=t):
                S.act(rs1[:], mv1[:, 1:2], AF.Ln, bias=LN_EPS)
                S.act(rs1[:], rs1[:], AF.Exp, scale=-0.5)
                S.ts("dve", hp[:], hp[:], mv1[:, 0:1], rs1[:, 0:1], ALU.subtract, ALU.mult)
                S.tt("pool", hp[:], hp[:], g1_bc[:], ALU.mult)
                S.tt("pool", hp[:], hp[:], b1_bc[:], ALU.add)
                S.dma("sp", h1_d[t * 128:(t + 1) * 128, :], hp[:], writes=[], scat=[h1_d])
            if ln_delay > 0:
                defer(ln_delay, ln_part)
            else:
                ln_part()

        load_cs(cs2[0], 0)
        emit_qproj(0, 0)
        sidx = 0
        nsteps = NT + LA
        for i, (qb, h) in enumerate(seq):
            nxt = seq[i + 1] if i + 1 < len(seq) else None
            prv = seq[i - 1] if i > 0 else None
            qt = qT_h[h % 2]
            po = O_[h % 2]
            ring = []
            for step in range(nsteps):
                if step == 0 and h == 0 and qb + 1 < 8:
                    load_cs(cs2[(qb + 1) % 2], qb + 1)
                if step == 0 and h == 2:
                    load_mix_inputs(qb)
                if step < NT:
                    kt = step
                    pss_ = S0[sidx % 3]
                    pt_ = pT[sidx % 3]
                    sidx += 1
                    S.mm(pss_[:], KT[0:96, h, kt * 128:(kt + 1) * 128], qt[0:96, :])
                    S.act(pt_[:], pss_[:], AF.Exp, scale=QK_SCALE)
                    ring.append((kt, pt_))
                if step >= LA:
                    kt2, pt2 = ring.pop(0)
                    S.mm(po[0:65, :], V_tm[:, kt2, h, :], pt2[:], start=(kt2 == 0), stop=(kt2 == NT - 1))
                if step == 1 and nxt is not None:
                    emit_qproj(*nxt)
                if step == 3 and prv is not None and not (qb >= 1 and h == 1):
                    emit_norm(*prv)
                if qb >= 1 and h in (0, 1) and step in (6, 19):
                    emit_mix_tile(qb - 1, 2 * h + (0 if step == 6 else 1), 6)
                if qb >= 1 and h == 1 and step == nsteps - 1:
                    emit_norm(qb, 0)
                tick()
        flush()
        emit_norm(7, 7)
        load_mix_inputs_done = True
        for j in range(4):
            emit_mix_tile(7, j, 0)

    if stop_after >= 3:
        A.reset(base_mark)
        wr_sb = A("wr_sb", [128, 8, 64], F32)
        rb_bc = A("rb_bc", [128, 64], F32)
        eoff_bc = A("eoff_bc", [128, 64], F32)
        tri_sb = A("tri_sb", [128, 128], BF16)
        wsg = A("wsg", [128, 8, 256], BF16)
        wsu = A("wsu", [128, 8, 256], BF16)
        wsd = A("wsd", [128, 2, 1024], BF16)
        zrow = A("zrow", [128, 1024], BF16)
        run_bf = A("run_bf", [128, 64], BF16)
        h1t = [A(f"h1t{i}", [128, 1024], F32) for i in range(3)]
        h1T_f = [A(f"h1T_f{i}", [128, 8, 128], F32) for i in range(3)]
        h1T_b = [A(f"h1T_b{i}", [128, 8, 512], BF16) for i in range(2)]
        h1b = [A(f"h1b{i}", [128, 1024], BF16) for i in range(4)]
        accs = [A(f"accs{i}", [128, 1024], F32) for i in range(2)]
        sgs = [A(f"sgs{i}", [128, 512], F32) for i in range(2)]
        hTs = A("hTs", [128, 2, 512], BF16)
        NR = 3
        R = []
        for i in range(NR):
            R.append({n: A(f"r_{n}{i}", [128, 64], F32) for n in
                      ("e1", "s", "b", "eq", "b2", "mb", "sel", "ws", "wgt", "cap", "key", "junk")})
            R[i].update({n: A(f"r_{n}{i}", [128, 8], F32) for n in
                         ("m1", "m2", "gs", "g8", "gm", "pen", "t8", "k8", "z", "slotf")})
            R[i]["den"] = A(f"r_den{i}", [128, 1], F32)
            R[i]["selb"] = A(f"r_selb{i}", [128, 64], BF16)

        S.dma("sp", wr_sb[:], w_router.rearrange("(k p) n -> p k n", p=128))
        S.dma("sp", rb_bc[:], router_bias.partition_broadcast(128))
        S.dma("sp", eoff_bc[:], eoff1_d.partition_broadcast(128))
        S.dma("sp", tri_sb[:], tri_d)
        S.dma("pool", wsg[:], w_sh_gate.rearrange("(k p) n -> p k n", p=128))
        S.dma("pool", wsu[:], w_sh_up.rearrange("(k p) n -> p k n", p=128))
        S.dma("pool", wsd[:], w_sh_down.rearrange("(m p) n -> p m n", p=128))
        S.memset("pool", zrow[:], 0.0)
        S.memset("pool", run_bf[:], 0.0)
        import os
        if not os.environ.get("NOZROW"):
            S.dma("sp", ybuf_d[NSLOT:NSLOT + 1, :], zrow[0:1, :], writes=[], scat=[ybuf_d])

        def load_h1(t):
            S.dma("sp", h1t[t % 3][:], h1_d[t * 128:(t + 1) * 128, :], reads=[h1_d])

        load_h1(0)
        load_h1(1)

        def shared_group(grp):
            hTb = h1T_b[grp % 2]
            for m in range(2):
                pg, pu_ = nb(), nb()
                for k in range(8):
                    S.mm(pg[:], wsg[:, k, m * 128:(m + 1) * 128], hTb[:, k, :], start=(k == 0), stop=(k == 7))
                for k in range(8):
                    S.mm(pu_[:], wsu[:, k, m * 128:(m + 1) * 128], hTb[:, k, :], start=(k == 0), stop=(k == 7))
                S.act(sgs[m][:], pg[:], AF.Silu)
                S.tt("dve", hTs[:, m, :], sgs[m][:], pu_[:], ALU.mult, scat=True)
                yield
            for jj in range(4):
                tt_ = grp * 4 + jj
                ac = accs[tt_ % 2]
                S.dma("sp", ac[:], h1_d[tt_ * 128:(tt_ + 1) * 128, :], reads=[h1_d])
                for half in range(2):
                    hc = slice(half * 512, (half + 1) * 512)
                    pd = nb()
                    for m in range(2):
                        S.mm(pd[:], hTs[:, m, jj * 128:(jj + 1) * 128], wsd[:, m, hc], start=(m == 0), stop=(m == 1))
                    S.stt("dve", ac[:, hc], ac[:, hc], ALPHA, pd[:], ALU.mult, ALU.add)
                    yield
                S.dma("sp", acc_d[tt_ * 128:(tt_ + 1) * 128, :], ac[:], writes=[], scat=[acc_d])

        def route(t):
            j = t % 4
            grp = t // 4
            ht = h1t[t % 3]
            hTf = h1T_f[t % 3]
            hTb = h1T_b[grp % 2]
            r = R[t % NR]
            pa, pb = nb(), nb()
            for k in range(8):
                dst = (pa if k < 4 else pb)[:, (k % 4) * 128:(k % 4 + 1) * 128]
                S.transpose(dst, ht[:, k * 128:(k + 1) * 128], identf[:])
            S.act(hTf[:, 0:4, :], pa[:].rearrange("p (k n) -> p k n", n=128), AF.Copy, scat=True)
            S.act(hTf[:, 4:8, :], pb[:].rearrange("p (k n) -> p k n", n=128), AF.Copy, scat=True)
            hb = h1b[t % 4]
            S.act(hb[:], ht[:], AF.Copy)
            if t + 2 < NT:
                load_h1(t + 2)
            pr = nb()
            for k in range(8):
                S.mm(pr[:, 0:64], hTf[:, k, :], wr_sb[:, k, :], start=(k == 0), stop=(k == 7))
            S.act(r["e1"][:], pr[:, 0:64], AF.Exp, scale=-1.0)
            yield
            S.copy("dve", hTb[:, :, j * 128:(j + 1) * 128], hTf[:], scat=True)
            yield
            S.ts("dve", r["s"][:], r["e1"][:], 1.0, None, ALU.add)
            yield
            S.op("dve", lambda e, r=r: e.reciprocal(r["s"][:], r["s"][:]), [r["s"][:]], [r["s"][:]])
            yield
            S.tt("dve", r["b"][:], r["s"][:], rb_bc[:], ALU.add)
            yield
            b3 = r["b"][:].rearrange("p (g i) -> p g i", i=8)
            S.reduce("dve", r["m1"][:], b3, ALU.max)
            yield
            S.tt("dve", r["eq"][:].rearrange("p (g i) -> p g i", i=8), b3,
                 r["m1"][:].unsqueeze(2).to_broadcast([128, 8, 8]), ALU.is_equal)
            yield
            S.stt("dve", r["b2"][:], r["eq"][:], -1.0e9, r["b"][:], ALU.mult, ALU.add)
            yield
            S.reduce("dve", r["m2"][:], r["b2"][:].rearrange("p (g i) -> p g i", i=8), ALU.max)
            yield
            S.tt("dve", r["gs"][:], r["m1"][:], r["m2"][:], ALU.add)
            yield
            S.op("dve", lambda e, r=r: e.max(out=r["g8"][:], in_=r["gs"][:]), [r["gs"][:]], [r["g8"][:]])
            yield
            S.ts("dve", r["gm"][:], r["gs"][:], r["g8"][:, 3:4], None, ALU.is_ge)
            yield
            S.ts("dve", r["pen"][:], r["gm"][:], 1.0, 1.0e9, ALU.subtract, ALU.mult)
            yield
            S.tt("dve", r["mb"][:].rearrange("p (g i) -> p g i", i=8), b3,
                 r["pen"][:].unsqueeze(2).to_broadcast([128, 8, 8]), ALU.add)
            yield
            S.op("dve", lambda e, r=r: e.max(out=r["t8"][:], in_=r["mb"][:]), [r["mb"][:]], [r["t8"][:]])
            yield
            S.ts("dve", r["sel"][:], r["mb"][:], r["t8"][:, 7:8], None, ALU.is_ge)
            yield
            S.act(r["selb"][:], r["sel"][:], AF.Copy)
            pc = nb()
            S.mm(pc[:, 0:64], tri_sb[:], r["selb"][:], start=True, stop=False)
            S.mm(pc[:, 0:64], ones_b[:], run_bf[:], start=False, stop=True)
            S.memset("dve", r["den"][:], 0.0)
            yield
            S.stt("dve", r["ws"][:], r["s"][:], 1.0, r["sel"][:], ALU.mult, ALU.mult, accum_out=r["den"][:])
            yield
            S.op("dve", lambda e, r=r: e.reciprocal(r["den"][:], r["den"][:]), [r["den"][:]], [r["den"][:]])
            yield
            S.ts("dve", r["wgt"][:], r["ws"][:], r["den"][:, 0:1], 2.5, ALU.mult, ALU.mult)
            yield
            S.tt("dve", run_bf[:], run_bf[:], r["selb"][:], ALU.add)
            yield
            S.ts("dve", r["cap"][:], pc[:, 0:64], float(C_CAP), None, ALU.is_lt)
            yield
            S.tt("dve", r["cap"][:], r["cap"][:], r["sel"][:], ALU.mult)
            yield
            S.tt("dve", r["key"][:], pc[:, 0:64], eoff_bc[:], ALU.add)
            yield
            S.tt("dve", r["key"][:], r["key"][:], r["cap"][:], ALU.mult)
            yield
            S.op("dve", lambda e, r=r: e.max(out=r["k8"][:], in_=r["key"][:]), [r["key"][:]], [r["k8"][:]])
            yield
            S.ts("dve", r["z"][:], r["k8"][:], 0.0, None, ALU.is_equal)
            yield
            S.stt("dve", r["slotf"][:], r["z"][:], float(NSLOT + 1), r["k8"][:], ALU.mult, ALU.add)
            yield
            S.ts("dve", r["slotf"][:], r["slotf"][:], -1.0, None, ALU.add)
            yield
            S.copy("dve", idx_all[:, t * 8:(t + 1) * 8], r["slotf"][:], scat=True)
            yield
            for k in range(8):
                S.op("pool", lambda e, t=t, k=k, hb=hb: e.indirect_dma_start(
                    out=xbuf_d, out_offset=bass.IndirectOffsetOnAxis(ap=idx_all[:, t * 8 + k:t * 8 + k + 1], axis=0),
                    in_=hb[:, :], in_offset=None, bounds_check=bc_reg(e), oob_is_err=False),
                    [hb[:], idx_all[:]], [], [xbuf_d], dma=True)
            for k in range(8):
                S.stt("dve", r["junk"][:], r["key"][:], r["k8"][:, k:k + 1], r["wgt"][:], ALU.is_equal, ALU.mult,
                      accum_out=wk_all[:, t * 8 + k:t * 8 + k + 1], scat_acc=True)
                yield

        LAG = 13
        active = []
        next_t = 0
        while next_t < NT or active:
            routes = [a for a in active if a[2] >= 0]
            if next_t < NT and (not routes or (len(routes) < 3 and routes[-1][1] >= LAG)):
                active.append([route(next_t), 0, next_t])
                next_t += 1
            for a in list(active):
                try:
                    next(a[0])
                    a[1] += 1
                except StopIteration:
                    active.remove(a)
                    if a[2] >= 0 and a[2] % 4 == 3:
                        active.append([shared_group(a[2] // 4), 10 ** 6, -1])
        if debug:
            S.dma("sp", idx_dbg, idx_all[:])
            S.dma("sp", wk_dbg, wk_all[:])

    if stop_after >= 4:
        A.reset(base_mark)
        Wg = [A(f"Wg{i}", [128, 8, 256], BF16) for i in range(2)]
        Wu = [A(f"Wu{i}", [128, 8, 256], BF16) for i in range(2)]
        Wd = [A(f"Wd{i}", [128, 2, 1024], BF16) for i in range(2)]
        XT = [A(f"XT{i}", [128, 8, C_CAP], BF16) for i in range(2)]
        sg4 = [A(f"sg4_{i}", [128, 512], F32) for i in range(2)]
        hT4 = [A(f"hT4_{i}", [128, 2, C_CAP], BF16) for i in range(2)]
        ysb = [A(f"ysb{i}", [128, 1024], BF16) for i in range(4)]

        def load_w(e):
            s = e % 2
            S.dma("pool", Wg[s][:], w_gate[e].rearrange("(k p) n -> p k n", p=128))
            S.dma("pool", Wu[s][:], w_up[e].rearrange("(k p) n -> p k n", p=128))
            S.dma("pool", Wd[s][:], w_down[e].rearrange("(m p) n -> p m n", p=128))

        def load_xt(e):
            s = e % 2
            for k in range(8):
                S.op("sp", lambda en, s=s, k=k, e=e: en.dma_start_transpose(
                    out=XT[s][:, k, :], in_=xbuf_d[e * C_CAP:(e + 1) * C_CAP, k * 128:(k + 1) * 128]),
                    [xbuf_d], [], [XT[s][:]], dma=True)

        load_w(0)
        load_xt(0)
        yi = 0
        for e in range(64):
            s = e % 2
            if e + 1 < 64:
                load_w(e + 1)
                load_xt(e + 1)
            hT = hT4[s]
            for (n0, nsz) in ((0, 512), (512, C_CAP - 512)):
                for m in range(2):
                    pg, pu_ = nb(), nb()
                    for k in range(8):
                        S.mm(pg[:, 0:nsz], Wg[s][:, k, m * 128:(m + 1) * 128], XT[s][:, k, n0:n0 + nsz], start=(k == 0), stop=(k == 7))
                    for k in range(8):
                        S.mm(pu_[:, 0:nsz], Wu[s][:, k, m * 128:(m + 1) * 128], XT[s][:, k, n0:n0 + nsz], start=(k == 0), stop=(k == 7))
                    sgt = sg4[m]
                    S.act(sgt[:, 0:nsz], pg[:, 0:nsz], AF.Silu)
                    S.tt("dve", hT[:, m, n0:n0 + nsz], sgt[:, 0:nsz], pu_[:, 0:nsz], ALU.mult, scat=True)
            for jt in range(C_CAP // 128):
                yb = ysb[yi % 4]
                yi += 1
                for half in range(2):
                    hc = slice(half * 512, (half + 1) * 512)
                    pd = nb()
                    for m in range(2):
                        S.mm(pd[:], hT[:, m, jt * 128:(jt + 1) * 128], Wd[s][:, m, hc], start=(m == 0), stop=(m == 1))
                    if half == 0:
                        S.act(yb[:, hc], pd[:], AF.Copy, scat=True)
                    else:
                        S.copy("dve", yb[:, hc], pd[:], scat=True)
                r0 = e * C_CAP + jt * 128
                S.dma("sp", ybuf_d[r0:r0 + 128, :], yb[:], writes=[], scat=[ybuf_d])

    if stop_after >= 5:
        A.reset(base_mark)
        g2_bc = A("g2_bc", [128, 1024], F32)
        b2_bc = A("b2_bc", [128, 1024], F32)
        acc5 = [A(f"acc5_{i}", [128, 1024], F32) for i in range(2)]
        acc5b = [A(f"acc5b_{i}", [128, 1024], F32) for i in range(2)]
        yg = [[A(f"yg{i}_{k}", [128, 1024], BF16) for k in range(8)] for i in range(2)]
        tmp5 = [A(f"tmp5_{i}", [128, 1024], F32) for i in range(4)]
        st5 = A("st5", [128, 12], F32)
        mv5 = A("mv5", [128, 2], F32)
        rs5 = A("rs5", [128, 1], F32)
        nb5 = A("nb5", [128, 1], F32)
        whi_b = A("whi_b", [128, 8], BF16)
        whi_f = A("whi_f", [128, 8], F32)
        wlo_f = A("wlo_f", [128, 8], F32)
        identb = A("identb", [128, 128], BF16)
        dgs = [A(f"dgs{i}", [128, 16, 128], BF16) for i in range(2)]
        S.copy("dve", identb[:], identf[:])
        S.dma("sp", g2_bc[:], ln2_g.partition_broadcast(128))
        S.dma("sp", b2_bc[:], ln2_b.partition_broadcast(128))

        def fetch(t):
            sl = t % 2
            S.dma("sp", acc5[sl][:], acc_d[t * 128:(t + 1) * 128, :], reads=[acc_d])
            for k in range(8):
                S.op("pool", lambda e, t=t, k=k, sl=sl: e.indirect_dma_start(
                    out=yg[sl][k][:, :], out_offset=None, in_=ybuf_d,
                    in_offset=bass.IndirectOffsetOnAxis(ap=idx_all[:, t * 8 + k:t * 8 + k + 1], axis=0),
                    bounds_check=bc_reg(e), oob_is_err=False),
                    [ybuf_d, idx_all[:]], [yg[sl][k][:]], dma=True)

        def premix(t):
            sl = t % 2
            wk = wk_all[:, t * 8:(t + 1) * 8]
            S.copy("dve", whi_b[:], wk)
            S.copy("dve", whi_f[:], whi_b[:])
            S.tt("dve", wlo_f[:], wk, whi_f[:], ALU.subtract)
            dg = dgs[t % 2]
            S.tt("dve", dg[:, 0:8, :], identb[:, None, :].to_broadcast([128, 8, 128]),
                 whi_f[:, :, None].to_broadcast([128, 8, 128]), ALU.mult, scat=True)
            S.tt("dve", dg[:, 8:16, :], identb[:, None, :].to_broadcast([128, 8, 128]),
                 wlo_f[:, :, None].to_broadcast([128, 8, 128]), ALU.mult, scat=True)
            banks = []
            for half in range(2):
                hc = slice(half * 512, (half + 1) * 512)
                pw = nb()
                for k in range(8):
                    S.mm(pw[:], dg[:, k, :], yg[sl][k][:, hc], start=(k == 0), stop=False)
                for k in range(8):
                    S.mm(pw[:], dg[:, 8 + k, :], yg[sl][k][:, hc], start=False, stop=(k == 7))
                banks.append(pw)
            return banks

        def post(t, banks):
            sl = t % 2
            a = acc5[sl]
            for half in range(2):
                hc = slice(half * 512, (half + 1) * 512)
                S.tt("dve", a[:, hc], a[:, hc], banks[half][:], ALU.add)
            S.op("dve", lambda e, a=a: e.bn_stats(st5[:, 0:6], a[:, 0:512]), [a[:]], [st5[:]])
            S.op("dve", lambda e, a=a: e.bn_stats(st5[:, 6:12], a[:, 512:1024]), [a[:], st5[:]], [st5[:]])
            S.op("dve", lambda e: e.bn_aggr(mv5[:], st5[:]), [st5[:]], [mv5[:]])
            S.act(rs5[:], mv5[:, 1:2], AF.Ln, bias=LN_EPS)
            S.act(rs5[:], rs5[:], AF.Exp, scale=-0.5)
            S.stt("dve", nb5[:], mv5[:, 0:1], -1.0, rs5[:], ALU.mult, ALU.mult)
            S.act(a[:], a[:], AF.Identity, bias=nb5[:], scale=rs5[:])
            S.tt("dve", a[:], a[:], g2_bc[:], ALU.mult)
            S.tt("dve", a[:], a[:], b2_bc[:], ALU.add)
            S.dma("sp", out[t * 128:(t + 1) * 128, :], a[:], writes=[], scat=[out])

        fetch(0)
        fetch(1)
        cur = premix(0)
        for t in range(NT):
            nxt_b = premix(t + 1) if t + 1 < NT else None
            post(t, cur)
            if t + 2 < NT:
                fetch(t + 2)
            cur = nxt_b

    S.emit()
    return nc, S, A


_CACHE = {}


def kernel(**inputs):
    consts = _host_constants()
    if "nc" not in _CACHE:
        _CACHE["nc"] = build()[0]
    nc = _CACHE["nc"]
    x = np.ascontiguousarray(inputs["x"], dtype=np.float32)
    shared = {}
    for k, v in inputs.items():
        if k == "x":
            continue
        v = np.ascontiguousarray(v)
        shared[k] = v.reshape(v.shape[1:])
    shared.update(consts)
    in_maps = []
    for b in range(8):
        m = dict(shared)
        m["x"] = x[b]
        in_maps.append(m)
    res = run_bass_kernel_spmd(nc, in_maps, core_ids=list(range(8)))
    return np.stack([np.asarray(r["out"]) for r in res.results], axis=0).astype(np.float32)
```

```python
import numpy as np
import ml_dtypes
import concourse.bass as bass
import concourse.mybir as mybir
from concourse.bass_utils import run_bass_kernel_spmd

F32 = mybir.dt.float32
BF16 = mybir.dt.bfloat16
I32 = mybir.dt.int32
AF = mybir.ActivationFunctionType
ALU = mybir.AluOpType
AX = mybir.AxisListType

ENGS = ("pe", "act", "dve", "pool", "sp")
NDMASEM = 8
SEM_ROLL = 12000

S_LEN = 4096
DM = 1024
NT = S_LEN // 128
C_CAP = 768
NSLOT = 64 * C_CAP
ALPHA = 2.0 ** 0.25
QK_SCALE = 96.0 ** -0.5
LN_EPS = 1e-5
RMS_EPS = 1e-6
POOL_WINDOWS = (2, 4, 8, 16)


class _Buf:
    __slots__ = ("ws", "rs")

    def __init__(self):
        self.ws = []
        self.rs = []


class Sched:
    def __init__(self, nc):
        self.nc = nc
        self.ops = []
        self.streams = {e: [] for e in ENGS}
        self.bufs = {}

    def _buf(self, x):
        name = x if isinstance(x, str) else x.tensor.name
        b = self.bufs.get(name)
        if b is None:
            b = self.bufs[name] = _Buf()
        return b

    def alias(self, new_name, old_names):
        b = self._buf(new_name)
        for o in old_names:
            ob = self.bufs.get(o)
            if ob is None:
                continue
            b.ws = list(dict.fromkeys(b.ws + ob.ws))
            b.rs = list(dict.fromkeys(b.rs + ob.rs))

    def op(self, eng, fn, reads=(), writes=(), scat=(), dma=False):
        i = len(self.ops)
        deps = set()

        def uniq(xs):
            out = []
            for x in xs:
                b = self._buf(x)
                if not any(b is o for o in out):
                    out.append(b)
            return out

        rb, wb, sb = uniq(reads), uniq(writes), uniq(scat)
        for b in rb:
            deps.update(b.ws)
        for b in wb:
            deps.update(b.ws)
            deps.update(b.rs)
        for b in sb:
            deps.update(b.rs)
        for b in rb:
            if not dma:
                b.rs = [j for j in b.rs if self.ops[j][3] or self.ops[j][0] != eng]
            b.rs.append(i)
        for b in wb:
            b.ws = [i]
            b.rs = []
        for b in sb:
            if not dma:
                b.ws = [j for j in b.ws if self.ops[j][3] or self.ops[j][0] != eng]
            b.ws.append(i)
        deps.discard(i)
        self.ops.append([eng, fn, deps, dma, False, None])
        self.streams[eng].append(i)
        return i

    def dma(self, q, out, in_, reads=None, writes=None, scat=(), **kw):
        r = [in_] if reads is None else reads
        w = [out] if writes is None else writes
        return self.op(q, lambda e: e.dma_start(out=out, in_=in_, **kw), r, w, scat, dma=True)

    def dmaT(self, q, out, in_):
        return self.op(q, lambda e: e.dma_start_transpose(out=out, in_=in_), [in_], [out], dma=True)

    def mm(self, out, lhsT, rhs, start=True, stop=True):
        return self.op("pe", lambda e: e.matmul(out, lhsT, rhs, start=start, stop=stop), [lhsT, rhs], [out])

    def transpose(self, out, in_, ident):
        return self.op("pe", lambda e: e.transpose(out, in_, ident), [in_, ident], [out])

    def act(self, out, in_, func, bias=None, scale=None, accum_out=None, scat=False):
        kw = {}
        r = [in_]
        w = [out]
        if bias is not None:
            kw["bias"] = bias
            if not isinstance(bias, (int, float)):
                r.append(bias)
        if scale is not None:
            kw["scale"] = scale
            if not isinstance(scale, (int, float)):
                r.append(scale)
        if accum_out is not None:
            kw["accum_out"] = accum_out
            w.append(accum_out)
        if scat:
            return self.op("act", lambda e: e.activation(out, in_, func, **kw), r, [], w)
        return self.op("act", lambda e: e.activation(out, in_, func, **kw), r, w)

    def tt(self, eng, out, in0, in1, op, scat=False):
        if scat:
            return self.op(eng, lambda e: e.tensor_tensor(out, in0, in1, op), [in0, in1], [], [out])
        return self.op(eng, lambda e: e.tensor_tensor(out, in0, in1, op), [in0, in1], [out])

    def ts(self, eng, out, in0, s1, s2, op0, op1=None, accum_out=None, scat=False):
        r = [in0]
        w = [out]
        if not isinstance(s1, (int, float)):
            r.append(s1)
        if s2 is not None and not isinstance(s2, (int, float)):
            r.append(s2)
        kw = {}
        if op1 is not None:
            kw["op1"] = op1
        if accum_out is not None:
            kw["accum_out"] = accum_out
            w.append(accum_out)
        if scat:
            return self.op(eng, lambda e: e.tensor_scalar(out, in0, s1, s2, op0, **kw), r, [], w)
        return self.op(eng, lambda e: e.tensor_scalar(out, in0, s1, s2, op0, **kw), r, w)

    def stt(self, eng, out, in0, scalar, in1, op0, op1, accum_out=None, scat_acc=False):
        r = [in0, in1]
        if not isinstance(scalar, (int, float)):
            r.append(scalar)
        kw = {}
        w = [out]
        sc = []
        if accum_out is not None:
            kw["accum_out"] = accum_out
            if scat_acc:
                sc.append(accum_out)
            else:
                w.append(accum_out)
        return self.op(eng, lambda e: e.scalar_tensor_tensor(out, in0, scalar, in1, op0, op1, **kw), r, w, sc)

    def copy(self, eng, out, in_, scat=False):
        if eng == "act":
            fn = lambda e: e.copy(out, in_)
        else:
            fn = lambda e: e.tensor_copy(out, in_)
        if scat:
            return self.op(eng, fn, [in_], [], [out])
        return self.op(eng, fn, [in_], [out])

    def memset(self, eng, ap, val):
        return self.op(eng, lambda e: e.memset(ap, val), [], [ap])

    def reduce(self, eng, out, in_, op, axis=AX.X):
        return self.op(eng, lambda e: e.tensor_reduce(out, in_, axis, op), [in_], [out])

    def emit(self):
        nc = self.nc
        ops = self.ops
        for o in ops:
            for d in o[2]:
                p = ops[d]
                if p[3]:
                    continue
                if p[0] == "pe" and o[0] == "pe" and not o[3]:
                    continue
                p[4] = True
        dsem = {e: [nc.alloc_semaphore(f"dsem_{e}_{k}") for k in range(NDMASEM)] for e in ("sp", "act", "pool")}
        dcnt = {e: [0] * NDMASEM for e in dsem}
        drr = {e: 0 for e in dsem}
        prevdma = {}
        nsig = {}
        for e in ENGS:
            cur = None
            cnt = 0
            n = 0
            for i in self.streams[e]:
                o = ops[i]
                if o[3]:
                    k = drr[e]
                    drr[e] = (k + 1) % NDMASEM
                    prevdma[i] = (dsem[e][k], dcnt[e][k])
                    dcnt[e][k] += 16
                    o[5] = (dsem[e][k], dcnt[e][k])
                elif o[4]:
                    if cur is None or cnt >= SEM_ROLL:
                        cur = nc.alloc_semaphore(f"sem_{e}_{n}")
                        n += 1
                        cnt = 0
                    cnt += 1
                    o[5] = (cur, cnt)
            nsig[e] = (n, cnt)
        self.stats = dict(nops=len(ops), per_eng={e: len(self.streams[e]) for e in ENGS}, nsem=nsig)
        streams = self.streams

        def run(e, engobj):
            known = {}

            def wait(sem, val):
                if val <= 0 or known.get(sem.num, 0) >= val:
                    return
                engobj.wait_ge(sem, val)
                known[sem.num] = val

            for i in streams[e]:
                o = ops[i]
                for d in sorted(o[2]):
                    p = ops[d]
                    if p[5] is None:
                        continue
                    if (not p[3]) and p[0] == "pe" and e == "pe" and not o[3]:
                        continue
                    wait(*p[5])
                if o[3]:
                    wait(*prevdma[i])
                ins = o[1](engobj)
                if o[3]:
                    ins.then_inc(o[5][0], 16)
                elif o[4]:
                    ins.then_inc(o[5][0], 1)
            if e in dsem:
                for k in range(NDMASEM):
                    wait(dsem[e][k], dcnt[e][k])

        with nc.Block() as block:
            @block.sync
            def _(eng):
                run("sp", eng)

            @block.scalar
            def _(eng):
                run("act", eng)

            @block.vector
            def _(eng):
                run("dve", eng)

            @block.gpsimd
            def _(eng):
                run("pool", eng)

            @block.tensor
            def _(eng):
                run("pe", eng)


class Alloc:
    def __init__(self, nc, S):
        self.nc = nc
        self.S = S
        self.lo = (nc.sbuf_base + 63) // 64 * 64
        self.hi = nc.sbuf_top
        self.ptr = self.lo
        self.live = []
        self.peak = 0

    def __call__(self, name, shape, dtype):
        per = 1
        for s in shape[1:]:
            per *= s
        nbytes = per * {F32: 4, BF16: 2, I32: 4}[dtype]
        nbytes = (nbytes + 63) // 64 * 64
        start = self.ptr
        end = start + nbytes
        assert end <= self.hi, f"SBUF overflow allocating {name}: {end} > {self.hi}"
        t = self.nc.alloc_sbuf_tensor_at(name, list(shape), dtype, offset=start)
        old = [n for (s, e, n) in self.live if s < end and e > start]
        if old:
            self.S.alias(t.name, old)
        self.live.append((start, end, t.name))
        self.ptr = end
        self.peak = max(self.peak, end)
        return t

    def mark(self):
        return self.ptr

    def reset(self, m):
        self.ptr = m


def _host_constants():
    pos = np.arange(S_LEN, dtype=np.float32)
    inv_freq = (10000.0 ** (-np.arange(0, 32, 2, dtype=np.float32) / 32)).astype(np.float32)
    ang = pos[:, None] * inv_freq[None, :]
    cos = np.cos(ang).astype(np.float32).T
    sin = np.sin(ang).astype(np.float32).T
    cs = np.stack([np.concatenate([cos, cos], 0), np.concatenate([-sin, sin], 0)], 0).astype(np.float32)

    band = np.zeros((4, 5, 128, 128), np.float32)

    def blk(w, t_src, t_dst):
        left = w // 2
        right = w - 1 - left
        s = np.arange(t_dst * 128, t_dst * 128 + 128)
        sp = np.arange(t_src * 128, t_src * 128 + 128)
        lo = np.clip(s - left, 0, S_LEN)
        hi = np.clip(s + right + 1, 0, S_LEN)
        cntv = (hi - lo).astype(np.float32)
        m = ((sp[:, None] >= lo[None, :]) & (sp[:, None] < hi[None, :])).astype(np.float32) / cntv[None, :]
        m = m - (sp[:, None] == s[None, :]).astype(np.float32)
        return m

    for g, w in enumerate(POOL_WINDOWS):
        band[g, 0] = blk(w, 4, 5)
        band[g, 1] = blk(w, 5, 5)
        band[g, 2] = blk(w, 6, 5)
        band[g, 3] = blk(w, 0, 0)
        band[g, 4] = blk(w, NT - 1, NT - 1)
    tri = np.triu(np.ones((128, 128), np.float32), 1)
    eoff1 = (np.arange(64, dtype=np.float32) * C_CAP + 1.0).astype(np.float32)
    return {
        "cs": cs,
        "band": band.astype(ml_dtypes.bfloat16),
        "identf": np.eye(128, dtype=np.float32),
        "tri": tri.astype(ml_dtypes.bfloat16),
        "eoff1": eoff1,
    }


def build(debug=False, stop_after=99):
    nc = bass.Bass("TRN2", target_bir_lowering=False)

    def DI(name, shape, dt):
        return nc.dram_tensor(name, list(shape), dt, kind="ExternalInput").ap()

    def DS(name, shape, dt):
        return nc.dram_tensor(name, list(shape), dt, kind=("ExternalOutput" if debug else "Internal")).ap()

    x = DI("x", [S_LEN, DM], F32)
    w_in = DI("w_in", [DM, 928], F32)
    w_pool = DI("w_pool", [4, 128, 128], F32)
    pool_scale = DI("pool_scale", [512], F32)
    q_norm_g = DI("q_norm_g", [256], F32)
    w_q_up = DI("w_q_up", [256, 768], F32)
    kv_norm_g = DI("kv_norm_g", [128], F32)
    w_kv_up = DI("w_kv_up", [128, 1024], F32)
    w_out = DI("w_out", [1024, 1024], F32)
    ln1_g = DI("ln1_g", [1024], F32)
    ln1_b = DI("ln1_b", [1024], F32)
    w_router = DI("w_router", [1024, 64], F32)
    router_bias = DI("router_bias", [64], F32)
    w_gate = DI("w_gate", [64, 1024, 256], F32)
    w_up = DI("w_up", [64, 1024, 256], F32)
    w_down = DI("w_down", [64, 256, 1024], F32)
    w_sh_gate = DI("w_sh_gate", [1024, 256], F32)
    w_sh_up = DI("w_sh_up", [1024, 256], F32)
    w_sh_down = DI("w_sh_down", [256, 1024], F32)
    ln2_g = DI("ln2_g", [1024], F32)
    ln2_b = DI("ln2_b", [1024], F32)
    cs_d = DI("cs", [2, 32, S_LEN], F32)
    band_d = DI("band", [4, 5, 128, 128], BF16)
    identf_d = DI("identf", [128, 128], F32)
    tri_d = DI("tri", [128, 128], BF16)
    eoff1_d = DI("eoff1", [64], F32)
    out = nc.dram_tensor("out", [S_LEN, DM], F32, kind="ExternalOutput").ap()

    yp_d = DS("yp_d", [4, 128, S_LEN], BF16)
    h1_d = DS("h1_d", [S_LEN, DM], F32)
    acc_d = DS("acc_d", [S_LEN, DM], F32)
    xbuf_d = nc.dram_tensor("xbuf_d", [NSLOT + 1, DM], BF16, kind="Internal").ap()
    ybuf_d = nc.dram_tensor("ybuf_d", [NSLOT + 1, DM], BF16, kind="Internal").ap()
    if debug:
        kt_dbg = DS("kt_dbg", [96, 8 * S_LEN], BF16)
        v_dbg = DS("v_dbg", [128, NT * 8 * 65], BF16)
        qn_dbg = DS("qn_dbg", [128, 2 * S_LEN], BF16)
        idx_dbg = DS("idx_dbg", [128, NT * 8], I32)
        wk_dbg = DS("wk_dbg", [128, NT * 8], F32)
        ym_dbg = DS("ym_dbg", [64, 8, S_LEN], BF16)
        mix_dbg = DS("mix_dbg", [S_LEN, DM], F32)

    S = Sched(nc)
    A = Alloc(nc, S)
    _bc = {}

    def bc_reg(e):
        if "r" not in _bc:
            r_ = e.alloc_register("bcreg")
            e.reg_mov(r_, NSLOT)
            _bc["r"] = r_
        return _bc["r"]
    ps = [nc.alloc_psum_tensor(f"ps{i}", [128, 512], F32) for i in range(8)]
    rr = [0]

    def nb():
        p = ps[rr[0] % 8]
        rr[0] += 1
        return p

    identf = A("identf_sb", [128, 128], F32)
    ones_f = A("ones_f", [128, 128], F32)
    ones_b = A("ones_b", [128, 128], BF16)
    idx_all = A("idx_all", [128, NT * 8], I32)
    wk_all = A("wk_all", [128, NT * 8], F32)
    S.dma("sp", identf[:], identf_d)
    S.memset("pool", ones_f[:], 1.0)
    S.memset("pool", ones_b[:], 1.0)
    S.memset("pool", wk_all[:], 0.0)
    zt = A("zt", [128, 1024], BF16)
    S.memset("pool", zt[:], 0.0)
    base_mark = A.mark()
    zf = [0]

    def zero_fill(n):
        for _ in range(n):
            r0 = zf[0] * 128
            if r0 >= NSLOT:
                return
            S.dma("pool", xbuf_d[r0:r0 + 128, :], zt[:], writes=[], scat=[xbuf_d])
            zf[0] += 1

    KT = A("KT", [128, 8, S_LEN], BF16)
    V_tm = A("V_tm", [128, NT, 8, 65], BF16)
    qnT = A("qnT", [128, 2, S_LEN], BF16)
    wq_sb = A("wq_sb", [128, 2, 768], BF16)
    wq_sw = A("wq_sw", [128, 2, 768], BF16)
    S.memset("pool", V_tm[:], 1.0)
    att_mark = A.mark()

    w_in_sb = A("w_in_sb", [128, 8, 928], BF16)
    wsw = A("wsw", [128, 8, 96], BF16)
    wpool_sb = A("wpool_sb", [128, 4, 128], BF16)
    pscale = A("pscale", [128, 4], F32)
    band_sb = A("band_sb", [128, 20, 128], BF16)
    wkv_sb = A("wkv_sb", [128, 1024], BF16)
    stg = A("stg", [128, 2, 768], F32)
    gq = A("gq", [128, 2], F32)
    gkv = A("gkv", [128, 1], F32)
    xs = [A(f"xs{i}", [128, 1024], F32) for i in range(2)]
    xT = [A(f"xT{i}", [128, 8, 512], BF16) for i in range(1)]
    cs_sb = [A(f"cs_sb{i}", [128, 2, 512], F32) for i in range(1)]
    sq = [A(f"sq{i}", [128, 512], F32) for i in range(2)]
    rq = A("rq", [128, 512], F32)
    rkv = A("rkv", [128, 512], F32)
    kvnT = A("kvnT", [128, 512], BF16)
    u_tm = [A(f"u_tm{i}", [128, 512], BF16) for i in range(10)]
    pooledT = A("pooledT", [128, 4, 512], BF16)
    ypT_c = [A(f"ypT_c{i}", [128, 4, 512], BF16) for i in range(1)]
    t1 = A("t1", [128, 512], F32)
    t2 = A("t2", [128, 512], F32)

    S.dma("pool", w_in_sb[:], w_in.rearrange("(k p) n -> p k n", p=128))
    S.dma("pool", wpool_sb[:], w_pool.rearrange("g c d -> c g d"))
    S.dma("sp", band_sb[:], band_d.rearrange("g k a b -> a (g k) b"))
    for g in range(4):
        S.dma("sp", pscale[:, g:g + 1], pool_scale[g * 128:(g + 1) * 128].rearrange("(p o) -> p o", o=1),
              scat=[pscale[:]], writes=[])
    S.dma("sp", gkv[:], kv_norm_g.rearrange("(p o) -> p o", o=1))
    for k in range(2):
        S.dma("sp", gq[:, k:k + 1], q_norm_g[k * 128:(k + 1) * 128].rearrange("(p o) -> p o", o=1),
              scat=[gq[:]], writes=[])
    S.dma("sp", stg[:, 0, :], w_kv_up[:, 0:768], writes=[], scat=[stg[:]])
    S.dma("sp", stg[:, 1, 0:256], w_kv_up[:, 768:1024], writes=[], scat=[stg[:]])
    S.ts("dve", wkv_sb[:, 0:768], stg[:, 0, :], gkv[:, 0:1], None, ALU.mult, scat=True)
    S.ts("dve", wkv_sb[:, 768:1024], stg[:, 1, 0:256], gkv[:, 0:1], None, ALU.mult, scat=True)
    stg2 = stg
    S.dma("sp", stg2[:], w_q_up.rearrange("(k p) n -> p k n", p=128))
    for k in range(2):
        S.ts("dve", wq_sb[:, k, :], stg2[:, k, :], gq[:, k:k + 1], None, ALU.mult, scat=True)
    wq4 = wq_sb[:].rearrange("p k (h c) -> p k h c", c=96)
    wqs4 = wq_sw[:].rearrange("p k (h c) -> p k h c", c=96)
    for k in range(2):
        S.copy("pool", wqs4[:, k, :, 0:64], wq4[:, k, :, 0:64], scat=True)
        S.copy("pool", wqs4[:, k, :, 64:80], wq4[:, k, :, 80:96], scat=True)
        S.copy("pool", wqs4[:, k, :, 80:96], wq4[:, k, :, 64:80], scat=True)
    S.copy("pool", wsw[:, :, 0:64], w_in_sb[:, :, 832:896], scat=True)
    S.copy("pool", wsw[:, :, 64:80], w_in_sb[:, :, 912:928], scat=True)
    S.copy("pool", wsw[:, :, 80:96], w_in_sb[:, :, 896:912], scat=True)

    def load_x(t):
        S.dma("sp", xs[t % 2][:], x[t * 128:(t + 1) * 128, :])

    def load_cs(buf, c):
        for i in range(2):
            S.dma("sp", buf[64:96, i, :], cs_d[i, :, c * 512:(c + 1) * 512], writes=[], scat=[buf[:]])

    def band(g, kind):
        return band_sb[:, g * 5 + kind, :]

    def pool_chunk(cc):
        yc = ypT_c[0]
        for g in range(4):
            pp = nb()
            for j in range(4):
                t = 4 * cc + j
                srcs = []
                if t > 0:
                    srcs.append((t - 1, band(g, 0)))
                srcs.append((t, band(g, 3 if t == 0 else (4 if t == NT - 1 else 1))))
                if t < NT - 1:
                    srcs.append((t + 1, band(g, 2)))
                for i, (ts_, bm) in enumerate(srcs):
                    S.mm(pp[:, j * 128:(j + 1) * 128], u_tm[ts_ % 10][:, g * 128:(g + 1) * 128], bm,
                         start=(i == 0), stop=(i == len(srcs) - 1))
            S.copy("dve", pooledT[:, g, :], pp[:], scat=True)
            py = nb()
            S.mm(py[:], wpool_sb[:, g, :], pooledT[:, g, :])
            S.act(yc[:, g, :], py[:], AF.Copy, scale=pscale[:, g:g + 1], scat=True)
        S.dma("sp", yp_d.rearrange("g d s -> d g s")[:, :, cc * 512:(cc + 1) * 512], yc[:], writes=[], scat=[yp_d])

    load_x(0)
    load_x(1)
    for c in range(8):
        cols = slice(c * 512, (c + 1) * 512)
        xTc = xT[0]
        csb = cs_sb[0]
        load_cs(csb, c)
        for j in range(4):
            t = 4 * c + j
            xt = xs[t % 2]
            pa, pb = nb(), nb()
            for k in range(8):
                dst = (pa if k < 4 else pb)[:, (k % 4) * 128:(k % 4 + 1) * 128]
                S.transpose(dst, xt[:, k * 128:(k + 1) * 128], identf[:])
            S.act(xTc[:, 0:4, j * 128:(j + 1) * 128], pa[:].rearrange("p (k n) -> p k n", n=128), AF.Copy, scat=True)
            S.copy("dve", xTc[:, 4:8, j * 128:(j + 1) * 128], pb[:].rearrange("p (k n) -> p k n", n=128), scat=True)
            if t + 2 < NT:
                load_x(t + 2)
        pq0, pq1, pkv, prm, prs = nb(), nb(), nb(), nb(), nb()
        for pt, c0 in ((pq0, 512), (pq1, 640), (pkv, 768)):
            for k in range(8):
                S.mm(pt[:], w_in_sb[:, k, c0:c0 + 128], xTc[:, k, :], start=(k == 0), stop=(k == 7))
        for k in range(8):
            S.mm(prm[0:96, :], w_in_sb[:, k, 832:928], xTc[:, k, :], start=(k == 0), stop=(k == 7))
        for k in range(8):
            S.mm(prs[0:96, :], wsw[:, k, :], xTc[:, k, :], start=(k == 0), stop=(k == 7))
        S.act(sq[0][:], pq0[:], AF.Square)
        S.act(sq[1][:], pq1[:], AF.Square)
        pss = nb()
        S.mm(pss[:], ones_f[:], sq[0][:], start=True, stop=False)
        S.mm(pss[:], ones_f[:], sq[1][:], start=False, stop=True)
        S.act(rq[:], pss[:], AF.Ln, scale=1.0 / 256, bias=RMS_EPS)
        S.act(rq[:], rq[:], AF.Exp, scale=-0.5)
        S.tt("dve", qnT[:, 0, cols], pq0[:], rq[:], ALU.mult, scat=True)
        S.tt("dve", qnT[:, 1, cols], pq1[:], rq[:], ALU.mult, scat=True)
        S.act(sq[0][:], pkv[:], AF.Square)
        pss2 = nb()
        S.mm(pss2[:], ones_f[:], sq[0][:])
        S.act(rkv[:], pss2[:], AF.Ln, scale=1.0 / 128, bias=RMS_EPS)
        S.act(rkv[:], rkv[:], AF.Exp, scale=-0.5)
        S.tt("dve", kvnT[:], pkv[:], rkv[:], ALU.mult)
        S.tt("dve", t1[64:96, :], prm[64:96, :], csb[64:96, 0, :], ALU.mult)
        S.tt("dve", t2[64:96, :], prs[64:96, :], csb[64:96, 1, :], ALU.mult)
        S.tt("dve", t1[64:96, :], t1[64:96, :], t2[64:96, :], ALU.add)
        S.copy("pool", KT[64:96, :, cols], t1[64:96, None, :].to_broadcast([32, 8, 512]), scat=True)
        for h in range(8):
            pk = nb()
            S.mm(pk[0:64, :], wkv_sb[:, h * 128:h * 128 + 64], kvnT[:])
            if h % 2 == 0:
                S.act(KT[0:64, h, cols], pk[0:64, :], AF.Copy, scat=True)
            else:
                S.copy("dve", KT[0:64, h, cols], pk[0:64, :], scat=True)
        for j in range(4):
            t = 4 * c + j
            pv = nb()
            S.mm(pv[:], kvnT[:, j * 128:(j + 1) * 128],
                 wkv_sb[:].rearrange("p (h c) -> p h c", c=128)[:, :, 64:128])
            if j % 2 == 0:
                S.act(V_tm[:, t, :, 0:64], pv[:].rearrange("p (h c) -> p h c", c=64), AF.Copy, scat=True)
            else:
                S.copy("dve", V_tm[:, t, :, 0:64], pv[:].rearrange("p (h c) -> p h c", c=64), scat=True)
        for j in range(4):
            t = 4 * c + j
            pu = nb()
            for k in range(8):
                S.mm(pu[:], xTc[:, k, j * 128:(j + 1) * 128], w_in_sb[:, k, 0:512], start=(k == 0), stop=(k == 7))
            S.act(u_tm[t % 10][:], pu[:], AF.Copy)
        if c >= 1:
            pool_chunk(c - 1)
    pool_chunk(7)

    if debug:
        S.dma("sp", kt_dbg, KT[0:96, :, :].rearrange("p h s -> p (h s)"))
        S.dma("sp", v_dbg, V_tm[:].rearrange("p t h c -> p (t h c)"))
        S.dma("sp", qn_dbg, qnT[:].rearrange("p k s -> p (k s)"))

    if stop_after >= 2:
        A.reset(att_mark)
        wout_p = A("wout_p", [128, 4, 1024], BF16)
        wout_m = A("wout_m", [128, 8, 1024], BF16)
        g1_bc = A("g1_bc", [128, 1024], F32)
        b1_bc = A("b1_bc", [128, 1024], F32)
        cs2 = [A(f"cs2_{i}", [128, 2, 512], F32) for i in range(2)]
        qT_h = [A(f"qT_h{i}", [128, 512], BF16) for i in range(2)]
        pT = [A(f"pT{i}", [128, 512], BF16) for i in range(3)]
        tq1 = A("tq1", [128, 512], F32)
        tq2 = A("tq2", [128, 512], F32)
        rec = A("rec", [128, 512], F32)
        bcs = A("bcs", [128, 512], F32)
        ymT = [A(f"ymT{h}", [128, 512], BF16) for h in range(8)]
        ypT_l = A("ypT_l", [128, 4, 512], BF16)
        xs2 = [A(f"xs2_{i}", [128, 1024], F32) for i in range(2)]
        hpre = [A(f"hpre{i}", [128, 1024], F32) for i in range(2)]
        st1 = A("st1", [128, 12], F32)
        mv1 = A("mv1", [128, 2], F32)
        rs1 = A("rs1", [128, 1], F32)

        S.dma("pool", wout_p[:], w_out[0:512, :].rearrange("(g p) n -> p g n", p=128))
        S.dma("pool", wout_m[0:64, :, :], w_out[512:1024, :].rearrange("(h p) n -> p h n", p=64))
        S.dma("sp", g1_bc[:], ln1_g.partition_broadcast(128))
        S.dma("sp", b1_bc[:], ln1_b.partition_broadcast(128))

        S0 = [ps[0], ps[1], ps[2]]
        O_ = [ps[3], ps[4]]
        PA, PB, PC = ps[5], ps[6], ps[7]

        LA = 2
        seq = [(qb, h) for qb in range(8) for h in range(8)]
        deferred = []

        def defer(n, fn):
            deferred.append([n, fn])

        def tick():
            due = [d for d in deferred if d[0] <= 0]
            for d in due:
                deferred.remove(d)
            for d in deferred:
                d[0] -= 1
            for d in due:
                d[1]()

        def flush():
            while deferred:
                tick()

        def emit_qproj(qb, h):
            qcols = slice(qb * 512, (qb + 1) * 512)
            csq = cs2[qb % 2]
            qt = qT_h[h % 2]
            for k in range(2):
                S.mm(PA[0:96, :], wq_sb[:, k, h * 96:(h + 1) * 96], qnT[:, k, qcols], start=(k == 0), stop=(k == 1))
            for k in range(2):
                S.mm(PB[0:96, :], wq_sw[:, k, h * 96:(h + 1) * 96], qnT[:, k, qcols], start=(k == 0), stop=(k == 1))
            S.copy("dve", qt[0:64, :], PA[0:64, :], scat=True)
            S.tt("dve", tq1[64:96, :], PA[64:96, :], csq[64:96, 0, :], ALU.mult)
            S.tt("dve", tq2[64:96, :], PB[64:96, :], csq[64:96, 1, :], ALU.mult)
            S.tt("pool", qt[64:96, :], tq1[64:96, :], tq2[64:96, :], ALU.add, scat=True)

        def emit_norm(qb, h):
            qcols = slice(qb * 512, (qb + 1) * 512)
            po = O_[h % 2]
            S.op("dve", lambda e, po=po: e.reciprocal(rec[64:65, :], po[64:65, :]), [po[:]], [rec[:]])
            S.mm(PC[0:64, :], ones_f[64:65, 0:64], rec[64:65, :])
            S.copy("dve", bcs[0:64, :], PC[0:64, :])
            S.tt("dve", ymT[h][0:64, :], po[0:64, :], bcs[0:64, :], ALU.mult)
            if debug:
                S.dma("sp", ym_dbg[:, h, qcols], ymT[h][0:64, :], writes=[], scat=[ym_dbg])

        def load_mix_inputs(qb):
            qcols = slice(qb * 512, (qb + 1) * 512)
            S.dma("sp", ypT_l[:], yp_d.rearrange("g d s -> d g s")[:, :, qcols])
            S.dma("sp", xs2[0][:], x[(4 * qb) * 128:(4 * qb + 1) * 128, :])

        def emit_mix_tile(qb, j, ln_delay):
            t = 4 * qb + j
            xt = xs2[j % 2]
            if j + 1 < 4:
                S.dma("sp", xs2[(j + 1) % 2][:], x[(t + 1) * 128:(t + 2) * 128, :])
            hp = hpre[t % 2]
            for half, pm in ((0, PA), (1, PB)):
                hc = slice(half * 512, (half + 1) * 512)
                for g in range(4):
                    S.mm(pm[:], ypT_l[:, g, j * 128:(j + 1) * 128], wout_p[:, g, hc], start=(g == 0), stop=False)
                for h in range(8):
                    S.mm(pm[:], ymT[h][0:64, j * 128:(j + 1) * 128], wout_m[0:64, h, hc], start=False, stop=(h == 7))
                if debug:
                    S.copy("dve", rec[:], pm[:])
                    S.dma("sp", mix_dbg[t * 128:(t + 1) * 128, hc], rec[:], writes=[], scat=[mix_dbg])
                S.stt("dve", hp[:, hc], xt[:, hc], ALPHA, pm[:], ALU.mult, ALU.add)
            S.op("dve", lambda e, hp=hp: e.bn_stats(st1[:, 0:6], hp[:, 0:512]), [hp[:]], [st1[:]])
            S.op("dve", lambda e, hp=hp: e.bn_stats(st1[:, 6:12], hp[:, 512:1024]), [hp[:], st1[:]], [st1[:]])
            S.op("dve", lambda e: e.bn_aggr(mv1[:], st1[:]), [st1[:]], [mv1[:]])

            def ln_part(hp=hp, t=t):
                S.act(rs1[:], mv1[:, 1:2], AF.Ln, bias=LN_EPS)
                S.act(rs1[:], rs1[:], AF.Exp, scale=-0.5)
                S.ts("dve", hp[:], hp[:], mv1[:, 0:1], rs1[:, 0:1], ALU.subtract, ALU.mult)
                S.tt("pool", hp[:], hp[:], g1_bc[:], ALU.mult)
                S.tt("pool", hp[:], hp[:], b1_bc[:], ALU.add)
                S.dma("sp", h1_d[t * 128:(t + 1) * 128, :], hp[:], writes=[], scat=[h1_d])
            if ln_delay > 0:
                defer(ln_delay, ln_part)
            else:
                ln_part()

        load_cs(cs2[0], 0)
        emit_qproj(0, 0)
        sidx = 0
        nsteps = NT + LA
        for i, (qb, h) in enumerate(seq):
            nxt = seq[i + 1] if i + 1 < len(seq) else None
            prv = seq[i - 1] if i > 0 else None
            qt = qT_h[h % 2]
            po = O_[h % 2]
            ring = []
            for step in range(nsteps):
                if step == 0 and h == 0 and qb + 1 < 8:
                    load_cs(cs2[(qb + 1) % 2], qb + 1)
                if step == 0 and h == 2:
                    load_mix_inputs(qb)
                if step < NT:
                    kt = step
                    pss_ = S0[sidx % 3]
                    pt_ = pT[sidx % 3]
                    sidx += 1
                    S.mm(pss_[:], KT[0:96, h, kt * 128:(kt + 1) * 128], qt[0:96, :])
                    S.act(pt_[:], pss_[:], AF.Exp, scale=QK_SCALE)
                    ring.append((kt, pt_))
                if step >= LA:
                    kt2, pt2 = ring.pop(0)
                    S.mm(po[0:65, :], V_tm[:, kt2, h, :], pt2[:], start=(kt2 == 0), stop=(kt2 == NT - 1))
                if step == 1 and nxt is not None:
                    emit_qproj(*nxt)
                if step in (5, 10, 15, 20, 25, 30):
                    zero_fill(1)
                if step == 3 and prv is not None and not (qb >= 1 and h == 1):
                    emit_norm(*prv)
                if qb >= 1 and h in (0, 1) and step in (6, 19):
                    emit_mix_tile(qb - 1, 2 * h + (0 if step == 6 else 1), 6)
                if qb >= 1 and h == 1 and step == nsteps - 1:
                    emit_norm(qb, 0)
                tick()
        flush()
        emit_norm(7, 7)
        zero_fill(NSLOT // 128)
        for j in range(4):
            emit_mix_tile(7, j, 0)

    if stop_after >= 3:
        A.reset(base_mark)
        wr_sb = A("wr_sb", [128, 8, 64], F32)
        rb_bc = A("rb_bc", [128, 64], F32)
        eoff_bc = A("eoff_bc", [128, 64], F32)
        tri_sb = A("tri_sb", [128, 128], BF16)
        wsg = A("wsg", [128, 8, 256], BF16)
        wsu = A("wsu", [128, 8, 256], BF16)
        wsd = A("wsd", [128, 2, 1024], BF16)
        zrow = A("zrow", [128, 1024], BF16)
        run_bf = A("run_bf", [128, 64], BF16)
        h1t = [A(f"h1t{i}", [128, 1024], F32) for i in range(3)]
        h1T_f = [A(f"h1T_f{i}", [128, 8, 128], F32) for i in range(3)]
        h1T_b = [A(f"h1T_b{i}", [128, 8, 512], BF16) for i in range(2)]
        h1b = [A(f"h1b{i}", [128, 1024], BF16) for i in range(4)]
        accs = [A(f"accs{i}", [128, 1024], F32) for i in range(2)]
        sgs = [A(f"sgs{i}", [128, 512], F32) for i in range(2)]
        hTs = A("hTs", [128, 2, 512], BF16)
        NR = 3
        R = []
        for i in range(NR):
            R.append({n: A(f"r_{n}{i}", [128, 64], F32) for n in
                      ("e1", "s", "b", "eq", "b2", "mb", "sel", "ws", "wgt", "cap", "key", "junk")})
            R[i].update({n: A(f"r_{n}{i}", [128, 8], F32) for n in
                         ("m1", "m2", "gs", "g8", "gm", "pen", "t8", "k8", "z", "slotf")})
            R[i]["den"] = A(f"r_den{i}", [128, 1], F32)
            R[i]["selb"] = A(f"r_selb{i}", [128, 64], BF16)

        S.dma("sp", wr_sb[:], w_router.rearrange("(k p) n -> p k n", p=128))
        S.dma("sp", rb_bc[:], router_bias.partition_broadcast(128))
        S.dma("sp", eoff_bc[:], eoff1_d.partition_broadcast(128))
        S.dma("sp", tri_sb[:], tri_d)
        S.dma("pool", wsg[:], w_sh_gate.rearrange("(k p) n -> p k n", p=128))
        S.dma("pool", wsu[:], w_sh_up.rearrange("(k p) n -> p k n", p=128))
        S.dma("pool", wsd[:], w_sh_down.rearrange("(m p) n -> p m n", p=128))
        S.memset("pool", zrow[:], 0.0)
        S.dma("sp", xbuf_d[NSLOT:NSLOT + 1, :], zrow[0:1, :], writes=[], scat=[xbuf_d])
        S.dma("sp", zrow[1:2, 0:64], xbuf_d[0:1, 0:64], reads=[xbuf_d])
        S.memset("pool", run_bf[:], 0.0)
        import os
        if not os.environ.get("NOZROW"):
            S.dma("sp", ybuf_d[NSLOT:NSLOT + 1, :], zrow[0:1, :], writes=[], scat=[ybuf_d])

        def load_h1(t):
            S.dma("sp", h1t[t % 3][:], h1_d[t * 128:(t + 1) * 128, :], reads=[h1_d])

        load_h1(0)
        load_h1(1)

        def shared_group(grp):
            hTb = h1T_b[grp % 2]
            for m in range(2):
                pg, pu_ = nb(), nb()
                for k in range(8):
                    S.mm(pg[:], wsg[:, k, m * 128:(m + 1) * 128], hTb[:, k, :], start=(k == 0), stop=(k == 7))
                for k in range(8):
                    S.mm(pu_[:], wsu[:, k, m * 128:(m + 1) * 128], hTb[:, k, :], start=(k == 0), stop=(k == 7))
                S.act(sgs[m][:], pg[:], AF.Silu)
                S.tt("dve", hTs[:, m, :], sgs[m][:], pu_[:], ALU.mult, scat=True)
                yield
            for jj in range(4):
                tt_ = grp * 4 + jj
                ac = accs[tt_ % 2]
                S.dma("sp", ac[:], h1_d[tt_ * 128:(tt_ + 1) * 128, :], reads=[h1_d])
                for half in range(2):
                    hc = slice(half * 512, (half + 1) * 512)
                    pd = nb()
                    for m in range(2):
                        S.mm(pd[:], hTs[:, m, jj * 128:(jj + 1) * 128], wsd[:, m, hc], start=(m == 0), stop=(m == 1))
                    S.stt("dve", ac[:, hc], ac[:, hc], ALPHA, pd[:], ALU.mult, ALU.add)
                    yield
                S.dma("sp", acc_d[tt_ * 128:(tt_ + 1) * 128, :], ac[:], writes=[], scat=[acc_d])

        def route(t):
            j = t % 4
            grp = t // 4
            ht = h1t[t % 3]
            hTf = h1T_f[t % 3]
            hTb = h1T_b[grp % 2]
            r = R[t % NR]
            pa, pb = nb(), nb()
            for k in range(8):
                dst = (pa if k < 4 else pb)[:, (k % 4) * 128:(k % 4 + 1) * 128]
                S.transpose(dst, ht[:, k * 128:(k + 1) * 128], identf[:])
            S.act(hTf[:, 0:4, :], pa[:].rearrange("p (k n) -> p k n", n=128), AF.Copy, scat=True)
            S.act(hTf[:, 4:8, :], pb[:].rearrange("p (k n) -> p k n", n=128), AF.Copy, scat=True)
            hb = h1b[t % 4]
            S.act(hb[:], ht[:], AF.Copy)
            if t + 2 < NT:
                load_h1(t + 2)
            pr = nb()
            for k in range(8):
                S.mm(pr[:, 0:64], hTf[:, k, :], wr_sb[:, k, :], start=(k == 0), stop=(k == 7))
            S.act(r["e1"][:], pr[:, 0:64], AF.Exp, scale=-1.0)
            yield
            S.copy("dve", hTb[:, :, j * 128:(j + 1) * 128], hTf[:], scat=True)
            yield
            S.ts("dve", r["s"][:], r["e1"][:], 1.0, None, ALU.add)
            yield
            S.op("dve", lambda e, r=r: e.reciprocal(r["s"][:], r["s"][:]), [r["s"][:]], [r["s"][:]])
            yield
            S.tt("dve", r["b"][:], r["s"][:], rb_bc[:], ALU.add)
            yield
            b3 = r["b"][:].rearrange("p (g i) -> p g i", i=8)
            S.reduce("dve", r["m1"][:], b3, ALU.max)
            yield
            S.tt("dve", r["eq"][:].rearrange("p (g i) -> p g i", i=8), b3,
                 r["m1"][:].unsqueeze(2).to_broadcast([128, 8, 8]), ALU.is_equal)
            yield
            S.stt("dve", r["b2"][:], r["eq"][:], -1.0e9, r["b"][:], ALU.mult, ALU.add)
            yield
            S.reduce("dve", r["m2"][:], r["b2"][:].rearrange("p (g i) -> p g i", i=8), ALU.max)
            yield
            S.tt("dve", r["gs"][:], r["m1"][:], r["m2"][:], ALU.add)
            yield
            S.op("dve", lambda e, r=r: e.max(out=r["g8"][:], in_=r["gs"][:]), [r["gs"][:]], [r["g8"][:]])
            yield
            S.ts("dve", r["gm"][:], r["gs"][:], r["g8"][:, 3:4], None, ALU.is_ge)
            yield
            S.ts("dve", r["pen"][:], r["gm"][:], 1.0, 1.0e9, ALU.subtract, ALU.mult)
            yield
            S.tt("dve", r["mb"][:].rearrange("p (g i) -> p g i", i=8), b3,
                 r["pen"][:].unsqueeze(2).to_broadcast([128, 8, 8]), ALU.add)
            yield
            S.op("dve", lambda e, r=r: e.max(out=r["t8"][:], in_=r["mb"][:]), [r["mb"][:]], [r["t8"][:]])
            yield
            S.ts("dve", r["sel"][:], r["mb"][:], r["t8"][:, 7:8], None, ALU.is_ge)
            yield
            S.act(r["selb"][:], r["sel"][:], AF.Copy)
            pc = nb()
            S.mm(pc[:, 0:64], tri_sb[:], r["selb"][:], start=True, stop=False)
            S.mm(pc[:, 0:64], ones_b[:], run_bf[:], start=False, stop=True)
            S.memset("dve", r["den"][:], 0.0)
            yield
            S.stt("dve", r["ws"][:], r["s"][:], 1.0, r["sel"][:], ALU.mult, ALU.mult, accum_out=r["den"][:])
            yield
            S.op("dve", lambda e, r=r: e.reciprocal(r["den"][:], r["den"][:]), [r["den"][:]], [r["den"][:]])
            yield
            S.ts("dve", r["wgt"][:], r["ws"][:], r["den"][:, 0:1], 2.5, ALU.mult, ALU.mult)
            yield
            S.tt("dve", run_bf[:], run_bf[:], r["selb"][:], ALU.add)
            yield
            S.ts("dve", r["cap"][:], pc[:, 0:64], float(C_CAP), None, ALU.is_lt)
            yield
            S.tt("dve", r["cap"][:], r["cap"][:], r["sel"][:], ALU.mult)
            yield
            S.tt("dve", r["key"][:], pc[:, 0:64], eoff_bc[:], ALU.add)
            yield
            S.tt("dve", r["key"][:], r["key"][:], r["cap"][:], ALU.mult)
            yield
            S.op("dve", lambda e, r=r: e.max(out=r["k8"][:], in_=r["key"][:]), [r["key"][:]], [r["k8"][:]])
            yield
            S.ts("dve", r["z"][:], r["k8"][:], 0.0, None, ALU.is_equal)
            yield
            S.stt("dve", r["slotf"][:], r["z"][:], float(NSLOT + 1), r["k8"][:], ALU.mult, ALU.add)
            yield
            S.ts("dve", r["slotf"][:], r["slotf"][:], -1.0, None, ALU.add)
            yield
            S.copy("dve", idx_all[:, t * 8:(t + 1) * 8], r["slotf"][:], scat=True)
            yield
            for k in range(8):
                S.op("pool", lambda e, t=t, k=k, hb=hb: e.indirect_dma_start(
                    out=xbuf_d, out_offset=bass.IndirectOffsetOnAxis(ap=idx_all[:, t * 8 + k:t * 8 + k + 1], axis=0),
                    in_=hb[:, :], in_offset=None, bounds_check=bc_reg(e), oob_is_err=False),
                    [hb[:], idx_all[:]], [], [xbuf_d], dma=True)
            for k in range(8):
                S.stt("dve", r["junk"][:], r["key"][:], r["k8"][:, k:k + 1], r["wgt"][:], ALU.is_equal, ALU.mult,
                      accum_out=wk_all[:, t * 8 + k:t * 8 + k + 1], scat_acc=True)
                yield

        LAG = 13
        active = []
        next_t = 0
        while next_t < NT or active:
            routes = [a for a in active if a[2] >= 0]
            if next_t < NT and (not routes or (len(routes) < 3 and routes[-1][1] >= LAG)):
                active.append([route(next_t), 0, next_t])
                next_t += 1
            for a in list(active):
                try:
                    next(a[0])
                    a[1] += 1
                except StopIteration:
                    active.remove(a)
                    if a[2] >= 0 and a[2] % 4 == 3:
                        active.append([shared_group(a[2] // 4), 10 ** 6, -1])
        if debug:
            S.dma("sp", idx_dbg, idx_all[:])
            S.dma("sp", wk_dbg, wk_all[:])

    if stop_after >= 4:
        A.reset(base_mark)
        Wg = [A(f"Wg{i}", [128, 8, 256], BF16) for i in range(2)]
        Wu = [A(f"Wu{i}", [128, 8, 256], BF16) for i in range(2)]
        Wd = [A(f"Wd{i}", [128, 2, 1024], BF16) for i in range(2)]
        XT = [A(f"XT{i}", [128, 8, C_CAP], BF16) for i in range(2)]
        sg4 = [A(f"sg4_{i}", [128, 512], F32) for i in range(2)]
        hT4 = [A(f"hT4_{i}", [128, 2, C_CAP], BF16) for i in range(2)]
        ysb = [A(f"ysb{i}", [128, 1024], BF16) for i in range(4)]

        def load_w(e):
            s = e % 2
            S.dma("pool", Wg[s][:], w_gate[e].rearrange("(k p) n -> p k n", p=128))
            S.dma("pool", Wu[s][:], w_up[e].rearrange("(k p) n -> p k n", p=128))
            S.dma("pool", Wd[s][:], w_down[e].rearrange("(m p) n -> p m n", p=128))

        def load_xt(e):
            s = e % 2
            for k in range(8):
                S.op("sp", lambda en, s=s, k=k, e=e: en.dma_start_transpose(
                    out=XT[s][:, k, :], in_=xbuf_d[e * C_CAP:(e + 1) * C_CAP, k * 128:(k + 1) * 128]),
                    [xbuf_d], [], [XT[s][:]], dma=True)

        load_w(0)
        load_xt(0)
        yi = 0
        for e in range(64):
            s = e % 2
            if e + 1 < 64:
                load_w(e + 1)
                load_xt(e + 1)
            hT = hT4[s]
            for (n0, nsz) in ((0, 512), (512, C_CAP - 512)):
                for m in range(2):
                    pg, pu_ = nb(), nb()
                    for k in range(8):
                        S.mm(pg[:, 0:nsz], Wg[s][:, k, m * 128:(m + 1) * 128], XT[s][:, k, n0:n0 + nsz], start=(k == 0), stop=(k == 7))
                    for k in range(8):
                        S.mm(pu_[:, 0:nsz], Wu[s][:, k, m * 128:(m + 1) * 128], XT[s][:, k, n0:n0 + nsz], start=(k == 0), stop=(k == 7))
                    sgt = sg4[m]
                    S.act(sgt[:, 0:nsz], pg[:, 0:nsz], AF.Silu)
                    S.tt("dve", hT[:, m, n0:n0 + nsz], sgt[:, 0:nsz], pu_[:, 0:nsz], ALU.mult, scat=True)
            for jt in range(C_CAP // 128):
                yb = ysb[yi % 4]
                yi += 1
                for half in range(2):
                    hc = slice(half * 512, (half + 1) * 512)
                    pd = nb()
                    for m in range(2):
                        S.mm(pd[:], hT[:, m, jt * 128:(jt + 1) * 128], Wd[s][:, m, hc], start=(m == 0), stop=(m == 1))
                    if half == 0:
                        S.act(yb[:, hc], pd[:], AF.Copy, scat=True)
                    else:
                        S.copy("dve", yb[:, hc], pd[:], scat=True)
                r0 = e * C_CAP + jt * 128
                S.dma("sp", ybuf_d[r0:r0 + 128, :], yb[:], writes=[], scat=[ybuf_d])

    if stop_after >= 5:
        A.reset(base_mark)
        g2_bc = A("g2_bc", [128, 1024], F32)
        b2_bc = A("b2_bc", [128, 1024], F32)
        acc5 = [A(f"acc5_{i}", [128, 1024], F32) for i in range(2)]
        acc5b = [A(f"acc5b_{i}", [128, 1024], F32) for i in range(2)]
        yg = [[A(f"yg{i}_{k}", [128, 1024], BF16) for k in range(8)] for i in range(2)]
        tmp5 = [A(f"tmp5_{i}", [128, 1024], F32) for i in range(4)]
        st5 = A("st5", [128, 12], F32)
        mv5 = A("mv5", [128, 2], F32)
        rs5 = A("rs5", [128, 1], F32)
        nb5 = A("nb5", [128, 1], F32)
        whi_b = A("whi_b", [128, 8], BF16)
        whi_f = A("whi_f", [128, 8], F32)
        wlo_f = A("wlo_f", [128, 8], F32)
        identb = A("identb", [128, 128], BF16)
        dgs = [A(f"dgs{i}", [128, 16, 128], BF16) for i in range(2)]
        S.copy("dve", identb[:], identf[:])
        S.dma("sp", g2_bc[:], ln2_g.partition_broadcast(128))
        S.dma("sp", b2_bc[:], ln2_b.partition_broadcast(128))

        def fetch(t):
            sl = t % 2
            S.dma("sp", acc5[sl][:], acc_d[t * 128:(t + 1) * 128, :], reads=[acc_d])
            for k in range(8):
                S.op("pool", lambda e, t=t, k=k, sl=sl: e.indirect_dma_start(
                    out=yg[sl][k][:, :], out_offset=None, in_=ybuf_d,
                    in_offset=bass.IndirectOffsetOnAxis(ap=idx_all[:, t * 8 + k:t * 8 + k + 1], axis=0),
                    bounds_check=bc_reg(e), oob_is_err=False),
                    [ybuf_d, idx_all[:]], [yg[sl][k][:]], dma=True)

        def premix(t):
            sl = t % 2
            wk = wk_all[:, t * 8:(t + 1) * 8]
            S.copy("dve", whi_b[:], wk)
            S.copy("dve", whi_f[:], whi_b[:])
            S.tt("dve", wlo_f[:], wk, whi_f[:], ALU.subtract)
            dg = dgs[t % 2]
            S.tt("dve", dg[:, 0:8, :], identb[:, None, :].to_broadcast([128, 8, 128]),
                 whi_f[:, :, None].to_broadcast([128, 8, 128]), ALU.mult, scat=True)
            S.tt("dve", dg[:, 8:16, :], identb[:, None, :].to_broadcast([128, 8, 128]),
                 wlo_f[:, :, None].to_broadcast([128, 8, 128]), ALU.mult, scat=True)
            banks = []
            for half in range(2):
                hc = slice(half * 512, (half + 1) * 512)
                pw = nb()
                for k in range(8):
                    S.mm(pw[:], dg[:, k, :], yg[sl][k][:, hc], start=(k == 0), stop=False)
                for k in range(8):
                    S.mm(pw[:], dg[:, 8 + k, :], yg[sl][k][:, hc], start=False, stop=(k == 7))
                banks.append(pw)
            return banks

        def post(t, banks):
            sl = t % 2
            a = acc5[sl]
            for half in range(2):
                hc = slice(half * 512, (half + 1) * 512)
                S.tt("dve", a[:, hc], a[:, hc], banks[half][:], ALU.add)
            S.op("dve", lambda e, a=a: e.bn_stats(st5[:, 0:6], a[:, 0:512]), [a[:]], [st5[:]])
            S.op("dve", lambda e, a=a: e.bn_stats(st5[:, 6:12], a[:, 512:1024]), [a[:], st5[:]], [st5[:]])
            S.op("dve", lambda e: e.bn_aggr(mv5[:], st5[:]), [st5[:]], [mv5[:]])
            S.act(rs5[:], mv5[:, 1:2], AF.Ln, bias=LN_EPS)
            S.act(rs5[:], rs5[:], AF.Exp, scale=-0.5)
            S.stt("dve", nb5[:], mv5[:, 0:1], -1.0, rs5[:], ALU.mult, ALU.mult)
            S.act(a[:], a[:], AF.Identity, bias=nb5[:], scale=rs5[:])
            S.tt("dve", a[:], a[:], g2_bc[:], ALU.mult)
            S.tt("dve", a[:], a[:], b2_bc[:], ALU.add)
            S.dma("sp", out[t * 128:(t + 1) * 128, :], a[:], writes=[], scat=[out])

        fetch(0)
        fetch(1)
        cur = premix(0)
        for t in range(NT):
            nxt_b = premix(t + 1) if t + 1 < NT else None
            post(t, cur)
            if t + 2 < NT:
                fetch(t + 2)
            cur = nxt_b

    S.emit()
    return nc, S, A


_CACHE = {}


def kernel(**inputs):
    consts = _host_constants()
    if "nc" not in _CACHE:
        _CACHE["nc"] = build()[0]
    nc = _CACHE["nc"]
    x = np.ascontiguousarray(inputs["x"], dtype=np.float32)
    shared = {}
    for k, v in inputs.items():
        if k == "x":
            continue
        v = np.ascontiguousarray(v)
        shared[k] = v.reshape(v.shape[1:])
    shared.update(consts)
    in_maps = []
    for b in range(8):
        m = dict(shared)
        m["x"] = x[b]
        in_maps.append(m)
    res = run_bass_kernel_spmd(nc, in_maps, core_ids=list(range(8)))
    return np.stack([np.asarray(r["out"]) for r in res.results], axis=0).astype(np.float32)
```
